# Optimizing a Trainium2 kernel written in Bass

```python
import math
import jax, jax.numpy as jnp
from jax import lax
import numpy as np

D_MODEL = 1024
BATCH = 8
SEQ = 4096
DEPTH = 1

LRU_WIDTH = 1024
LRU_HEADS = 16
LRU_HEAD_DIM = LRU_WIDTH // LRU_HEADS
LRU_CONV = 4
LRU_C = 8.0
HYENA_WIDTH = 1024
HYENA_ORDER = 2
HYENA_CONV = 3
FILTER_EMB = 33
FILTER_BANDS = (FILTER_EMB - 1) // 2
FILTER_HIDDEN = 64
DECAY_FAST = 0.3
DECAY_SLOW = 1.5
DECAY_TARGET = 1e-2
N_BRANCHES = 2
IN_COLS = 2 * LRU_WIDTH + (HYENA_ORDER + 1) * HYENA_WIDTH + N_BRANCHES * D_MODEL
N_EXPERTS = 16
CAPACITY_FACTOR = 2
D_FF_EXPERT = 2 * D_MODEL
EPS = 1e-6

kernel_name = "hybrid_rglru_hyena_ecmoe_encoder"


def rmsnorm(x, g):
    xf = x.astype(jnp.float32)
    y = xf * lax.rsqrt(jnp.mean(xf * xf, axis=-1, keepdims=True) + EPS)
    return (y * g.astype(jnp.float32)).astype(x.dtype)


def dwconv(x, w, b, pad):
    C = x.shape[-1]
    y = lax.conv_general_dilated(x, w[:, None, :].astype(x.dtype), window_strides=(1,), padding=[pad],
                                 dimension_numbers=("NWC", "WIO", "NWC"), feature_group_count=C)
    return y + b.astype(x.dtype)


def rg_lru(xa, w_r, b_r, w_i, b_i, lam, reverse):
    B, S, W = xa.shape
    xh = xa.reshape(B, S, LRU_HEADS, LRU_HEAD_DIM)
    r = jax.nn.sigmoid(jnp.einsum('bshi,hij->bshj', xh, w_r.astype(jnp.float32)).reshape(B, S, W) + b_r.astype(jnp.float32))
    i = jax.nn.sigmoid(jnp.einsum('bshi,hij->bshj', xh, w_i.astype(jnp.float32)).reshape(B, S, W) + b_i.astype(jnp.float32))
    log_a = -LRU_C * r * jax.nn.softplus(-lam.astype(jnp.float32))
    a = jnp.exp(log_a)
    mult = jnp.sqrt(jnp.maximum(1.0 - jnp.exp(2.0 * log_a), 0.0))
    pos = jnp.arange(S)[None, :, None]
    start = S - 1 if reverse else 0
    mult = jnp.where(pos == start, 1.0, mult)
    bx = mult * (i * xa)

    def combine(e1, e2):
        a1, b1 = e1
        a2, b2 = e2
        return a1 * a2, a2 * b1 + b2

    _, h = lax.associative_scan(combine, (a, bx), axis=1, reverse=reverse)
    return h


def hyena_filters(L, w1, b1, w2, b2, w3, freq):
    f32 = jnp.float32
    t = jnp.linspace(0.0, 1.0, L, dtype=f32)[:, None]
    w = (2.0 * math.pi / L) * jnp.arange(L, dtype=f32)[:, None]
    f = jnp.linspace(1e-4, FILTER_BANDS - 1, FILTER_BANDS, dtype=f32)[None, :]
    z = jnp.concatenate([t, jnp.cos(w * f), -jnp.sin(w * f)], axis=-1)
    freq = freq.astype(f32)
    h = jnp.sin(freq[0] * (z @ w1.astype(f32) + b1.astype(f32)))
    h = jnp.sin(freq[1] * (h @ w2.astype(f32) + b2.astype(f32)))
    h = (h @ w3.astype(f32)).reshape(L, HYENA_ORDER, 2, HYENA_WIDTH)
    min_decay = math.log(DECAY_TARGET) / DECAY_SLOW
    max_decay = math.log(DECAY_TARGET) / DECAY_FAST
    deltas = jnp.abs(jnp.linspace(min_decay, max_decay, HYENA_WIDTH, dtype=f32))
    decay = jnp.exp(-t * deltas[None, :])
    return h * decay[:, None, None, :]


def bidir_kernel(hf, hb):
    zero = jnp.zeros((1, hf.shape[1]), hf.dtype)
    return jnp.concatenate([hf, zero, hb[:0:-1]], axis=0)


def fftconv(u, k, bias):
    L = u.shape[1]
    U = jnp.fft.rfft(u, n=2 * L, axis=1)
    K = jnp.fft.rfft(k, axis=0)
    y = jnp.fft.irfft(U * K[None], n=2 * L, axis=1)[:, :L]
    return y + u * bias.astype(jnp.float32)


def setup_inputs(seed: int = 0) -> dict:
    key = jax.random.key(seed)
    ks = jax.random.split(key, 32)
    f32 = jnp.float32
    nrm = lambda k, shape, s: (jax.random.normal(k, shape, f32) * s).astype(f32)
    Dp = DEPTH
    u = jax.random.uniform(ks[9], (Dp, 2, LRU_WIDTH), f32, 0.9, 0.999)
    a0 = u ** (1.0 / LRU_C)
    lam = jnp.log(a0) - jnp.log1p(-a0)
    return {
        "x": nrm(ks[0], (BATCH, SEQ, D_MODEL), 1.0),
        "g_mix": 1.0 + nrm(ks[1], (Dp, D_MODEL), 0.01),
        "w_in": nrm(ks[2], (Dp, D_MODEL, IN_COLS), D_MODEL ** -0.5),
        "conv_a_w": nrm(ks[3], (Dp, LRU_CONV, LRU_WIDTH), LRU_CONV ** -0.5),
        "conv_a_b": nrm(ks[4], (Dp, LRU_WIDTH), 0.01),
        "lru_w_r": nrm(ks[5], (Dp, 2, LRU_HEADS, LRU_HEAD_DIM, LRU_HEAD_DIM), LRU_HEAD_DIM ** -0.5),
        "lru_b_r": nrm(ks[6], (Dp, 2, LRU_WIDTH), 0.01),
        "lru_w_i": nrm(ks[7], (Dp, 2, LRU_HEADS, LRU_HEAD_DIM, LRU_HEAD_DIM), LRU_HEAD_DIM ** -0.5),
        "lru_b_i": nrm(ks[8], (Dp, 2, LRU_WIDTH), 0.01),
        "lru_lambda": lam,
        "w_a_out": nrm(ks[10], (Dp, LRU_WIDTH, D_MODEL), LRU_WIDTH ** -0.5),
        "conv_b_w": nrm(ks[11], (Dp, HYENA_CONV, (HYENA_ORDER + 1) * HYENA_WIDTH), HYENA_CONV ** -0.5),
        "conv_b_b": nrm(ks[12], (Dp, (HYENA_ORDER + 1) * HYENA_WIDTH), 0.01),
        "filt_w1": nrm(ks[13], (Dp, FILTER_EMB, FILTER_HIDDEN), FILTER_EMB ** -0.5),
        "filt_b1": nrm(ks[14], (Dp, FILTER_HIDDEN), 0.1),
        "filt_w2": nrm(ks[15], (Dp, FILTER_HIDDEN, FILTER_HIDDEN), FILTER_HIDDEN ** -0.5),
        "filt_b2": nrm(ks[16], (Dp, FILTER_HIDDEN), 0.1),
        "filt_w3": nrm(ks[17], (Dp, FILTER_HIDDEN, HYENA_ORDER * 2 * HYENA_WIDTH), 0.005),
        "filt_freq": 1.0 + nrm(ks[18], (Dp, 2, FILTER_HIDDEN), 0.01),
        "filt_bias": nrm(ks[19], (Dp, HYENA_ORDER, HYENA_WIDTH), 0.5),
        "w_b_out": nrm(ks[20], (Dp, HYENA_WIDTH, D_MODEL), HYENA_WIDTH ** -0.5),
        "w_o": nrm(ks[21], (Dp, D_MODEL, D_MODEL), D_MODEL ** -0.5),
        "g_ffn": 1.0 + nrm(ks[22], (Dp, D_MODEL), 0.01),
        "w_router": nrm(ks[23], (Dp, D_MODEL, N_EXPERTS), D_MODEL ** -0.5),
        "w_gate": nrm(ks[24], (Dp, N_EXPERTS, D_MODEL, D_FF_EXPERT), D_MODEL ** -0.5),
        "w_up": nrm(ks[25], (Dp, N_EXPERTS, D_MODEL, D_FF_EXPERT), D_MODEL ** -0.5),
        "w_down": nrm(ks[26], (Dp, N_EXPERTS, D_FF_EXPERT, D_MODEL), D_FF_EXPERT ** -0.5),
        "g_final": 1.0 + nrm(ks[27], (D_MODEL,), 0.01),
    }


def reference(x, g_mix, w_in, conv_a_w, conv_a_b, lru_w_r, lru_b_r, lru_w_i, lru_b_i, lru_lambda, w_a_out,
              conv_b_w, conv_b_b, filt_w1, filt_b1, filt_w2, filt_b2, filt_w3, filt_freq, filt_bias, w_b_out,
              w_o, g_ffn, w_router, w_gate, w_up, w_down, g_final):
    f32 = jnp.float32
    dt = x.dtype
    B, S, D = x.shape
    bidx = jnp.arange(B)[:, None, None]
    cap = CAPACITY_FACTOR * S // N_EXPERTS
    for l in range(DEPTH):
        h = rmsnorm(x, g_mix[l])
        proj = h @ w_in[l]
        a_x, a_gate, hy, gates = jnp.split(
            proj, [LRU_WIDTH, 2 * LRU_WIDTH, 2 * LRU_WIDTH + (HYENA_ORDER + 1) * HYENA_WIDTH], axis=-1)

        xa = dwconv(a_x, conv_a_w[l], conv_a_b[l], (LRU_CONV // 2, LRU_CONV - 1 - LRU_CONV // 2)).astype(f32)
        h_a = (rg_lru(xa, lru_w_r[l, 0], lru_b_r[l, 0], lru_w_i[l, 0], lru_b_i[l, 0], lru_lambda[l, 0], False)
               + rg_lru(xa, lru_w_r[l, 1], lru_b_r[l, 1], lru_w_i[l, 1], lru_b_i[l, 1], lru_lambda[l, 1], True))
        y_a = (h_a * jax.nn.gelu(a_gate.astype(f32))).astype(dt) @ w_a_out[l]

        hy = dwconv(hy, conv_b_w[l], conv_b_b[l], (HYENA_CONV // 2, HYENA_CONV - 1 - HYENA_CONV // 2))
        v, x1, x2 = jnp.split(hy, HYENA_ORDER + 1, axis=-1)
        filt = hyena_filters(S, filt_w1[l], filt_b1[l], filt_w2[l], filt_b2[l], filt_w3[l], filt_freq[l])
        z = v.astype(f32)
        for n, gate in enumerate((x1, x2)):
            k = bidir_kernel(filt[:, n, 0], filt[:, n, 1])
            z = gate.astype(f32) * fftconv(z, k, filt_bias[l, n])
        y_b = z.astype(dt) @ w_b_out[l]

        g_a, g_b = jnp.split(jax.nn.sigmoid(gates.astype(f32)), N_BRANCHES, axis=-1)
        merged = (g_a * y_a.astype(f32) + g_b * y_b.astype(f32)).astype(dt)
        x = x + merged @ w_o[l]

        h = rmsnorm(x, g_ffn[l])
        aff = jax.nn.softmax((h @ w_router[l]).astype(f32), axis=-1)
        vals, idx = lax.top_k(jnp.swapaxes(aff, 1, 2), cap)
        xe = h[bidx, idx]
        gt = jnp.einsum('becd,edf->becf', xe, w_gate[l])
        up = jnp.einsum('becd,edf->becf', xe, w_up[l])
        ye = jnp.einsum('becf,efd->becd', jax.nn.silu(gt) * up, w_down[l])
        ye = ye * vals[..., None].astype(ye.dtype)
        x = x + jnp.zeros_like(x).at[bidx, idx].add(ye.astype(dt))
    return rmsnorm(x, g_final)
```

```python
import math
import numpy as np
import ml_dtypes
from contextlib import ExitStack
import concourse.bass as bass
import concourse.mybir as mybir
from concourse.bass_utils import run_bass_kernel_spmd


ENGS = ("pe", "act", "dve", "pool", "sp")


class Prog:
    def __init__(self, nc, es):
        self.nc = nc
        self.es = es
        self.ops = {e: [] for e in ENGS}
        self.sems = {}
        self.keys = {}
        self.seen = {e: {} for e in ENGS}
        for e in ENGS:
            self._sem("E_" + e)

    def _sem(self, name):
        if name not in self.sems:
            self.sems[name] = [self.es.enter_context(self.nc.semaphore("s_" + name)), 0]
        return self.sems[name]

    def _record(self, eng, fn, r, w, signame, inc):
        need = {}

        def req(sv):
            if sv is None:
                return
            s, v = sv
            if eng == "pe" and s == "E_pe":
                return
            if need.get(s, 0) < v:
                need[s] = v
        for k in r:
            st = self.keys.get(k)
            if st:
                req(st[0])
        for k in w:
            st = self.keys.get(k)
            if st:
                req(st[0])
                for s, v in st[1].items():
                    req((s, v))
        if signame.startswith("D_") and signame in self.sems and self.sems[signame][1] > 0:
            req((signame, self.sems[signame][1]))
        waits = []
        seen = self.seen[eng]
        for s, v in need.items():
            if seen.get(s, 0) < v:
                seen[s] = v
                waits.append((self.sems[s][0], v))
        sem = self._sem(signame)
        sem[1] += inc
        val = sem[1]
        for k in r:
            st = self.keys.setdefault(k, [None, {}])
            st[1][signame] = val
        for k in w:
            self.keys[k] = [(signame, val), {}]
        self.ops[eng].append((waits, fn, sem[0], inc))

    def op(self, eng, fn, r=(), w=()):
        self._record(eng, fn, r, w, "E_" + eng, 1)

    def dma(self, eng, stream, fn, r=(), w=()):
        self._record(eng, fn, r, w, "D_" + stream, 16)

    def barrier(self):
        tot = {s: v[1] for s, v in self.sems.items() if v[1] > 0}
        for e in ENGS:
            waits = []
            seen = self.seen[e]
            for s, v in tot.items():
                if s == "E_" + e:
                    continue
                if seen.get(s, 0) < v:
                    seen[s] = v
                    waits.append((self.sems[s][0], v))
            if waits:
                self.ops[e].append((waits, None, None, 0))
        self.keys = {}

    def emit(self):
        nc = self.nc
        with nc.Block() as block:
            def run(name):
                def body(e):
                    for waits, fn, sem, inc in self.ops[name]:
                        for s, v in waits:
                            e.wait_ge(s, v)
                        if fn is not None:
                            fn(e).then_inc(sem, inc)
                return body
            block.sync(run("sp"))
            block.scalar(run("act"))
            block.vector(run("dve"))
            block.gpsimd(run("pool"))
            block.tensor(run("pe"))


BF=ml_dtypes.bfloat16
def make_consts(S=4096):
    L=S; N=2*S
    t=np.linspace(0.0,1.0,L,dtype=np.float32)[:,None]
    w=(np.float32(2.0*math.pi/L)*np.arange(L,dtype=np.float32))[:,None]
    f=np.linspace(1e-4,15,16,dtype=np.float32)[None,:]
    z=np.concatenate([t,np.cos(w*f),-np.sin(w*f)],axis=-1).astype(np.float32)
    c={}
    c["c_zT"]=np.ascontiguousarray(z.T)
    min_decay=math.log(1e-2)/1.5; max_decay=math.log(1e-2)/0.3
    deltas=np.abs(np.linspace(min_decay,max_decay,1024,dtype=np.float32))
    c["c_ndel"]=np.ascontiguousarray((-deltas).reshape(8,128).T).astype(np.float32)
    c["c_tn"]=np.tile(t[:512,0][None,:],(128,1)).astype(np.float32)
    thi=np.arange(64)[:,None,None]; tlo=np.arange(128)[None,:,None]; fa=np.arange(64)[None,None,:]
    ang=2*np.pi*((128*thi+tlo)*fa % N)/N
    c["c_FT1"]=np.concatenate([np.cos(ang),-np.sin(ang)],axis=2).astype(BF)
    a=np.arange(128)
    ang=2*np.pi*(np.outer(a,a)%128)/128
    c["c_G"]=np.stack([np.cos(ang),-np.sin(ang),np.sin(ang)],axis=1).astype(BF)
    fa=np.arange(64)[:,None,None]; tlo=np.arange(128)[None,:,None]; thi=np.arange(32)[None,None,:]
    ang=2*np.pi*((tlo+128*thi)*fa % N)/N
    c["c_MI"]=(np.concatenate([np.cos(ang),-np.sin(ang)],axis=0)/N).astype(BF)
    c["c_iota1"] = np.tile(np.arange(1, 513, dtype=np.float32)[None, :], (128, 1))
    tokv = (np.arange(32)[None, :] * 128 + np.arange(128)[:, None])
    c["c_tokab"] = np.stack([tokv // 64, tokv % 64], axis=-1).astype(np.float32)
    return c


F32 = mybir.dt.float32
BF16 = mybir.dt.bfloat16
I32 = mybir.dt.int32
ALU = mybir.AluOpType
AF = mybir.ActivationFunctionType

S = 4096
D = 1024
NCOL = 7168
EPS = 1e-6


def revap(ap, n):
    a = ap.ap
    return bass.AP(ap.tensor, ap.offset + (n - 1) * a[-1][0], [list(x) for x in a[:-1]] + [[-a[-1][0], n]])


class K:
    pass


def build(debug=False, upto=99):
    nc = bass.Bass("TRN2", target_bir_lowering=False)
    k = K()
    k.nc = nc
    dbgkind = "ExternalOutput" if debug else "Internal"

    def din(name, shape, dt=F32):
        return nc.dram_tensor(name, list(shape), dt, kind="ExternalInput").ap()

    x = din("x", [S, D])
    g_mix = din("g_mix", [1, D])
    w_in = din("w_in", [D, NCOL])
    conv_a_w = din("conv_a_w", [4, 1024])
    conv_a_b = din("conv_a_b", [1, 1024])
    lru_w_r = din("lru_w_r", [2, 16, 64, 64])
    lru_b_r = din("lru_b_r", [2, 1024])
    lru_w_i = din("lru_w_i", [2, 16, 64, 64])
    lru_b_i = din("lru_b_i", [2, 1024])
    lru_lambda = din("lru_lambda", [2, 1024])
    conv_b_w = din("conv_b_w", [3, 3072])
    conv_b_b = din("conv_b_b", [1, 3072])
    out = nc.dram_tensor("out", [S, D], F32, kind="ExternalOutput").ap()
    projT = nc.dram_tensor("projT", [NCOL, S], F32, kind=dbgkind).ap()
    maT = nc.dram_tensor("maT", [1024, S], BF16, kind=dbgkind).ap()
    mbT = nc.dram_tensor("mbT", [1024, S], BF16, kind=dbgkind).ap()
    Kspec = nc.dram_tensor("Kspec", [16, 128, 2, 128 * 64], BF16, kind=dbgkind).ap()
    H2dbg = nc.dram_tensor("H2dbg", [64, S], F32, kind=dbgkind).ap()
    filt_w1 = din("filt_w1", [33, 64]); filt_b1 = din("filt_b1", [64, 1]); filt_w2 = din("filt_w2", [64, 64]); filt_b2 = din("filt_b2", [64, 1])
    filt_w3 = din("filt_w3", [64, 4096]); filt_freq = din("filt_freq", [64, 2]); filt_bias = din("filt_bias", [2, 1024])
    w_a_out = din("w_a_out", [D, D]); w_b_out = din("w_b_out", [D, D]); w_o = din("w_o", [D, D])
    g_ffn = din("g_ffn", [1, D]); w_router = din("w_router", [D, 16]); g_final = din("g_final", [1, D])
    w_gate = din("w_gate", [16, D, 2048]); w_up = din("w_up", [16, D, 2048]); w_down = din("w_down", [16, 2048, D])
    c_iota1 = din("c_iota1", [128, 512]); c_tokab = din("c_tokab", [128, 32, 2])
    x1d = nc.dram_tensor("x1d", [S, D], F32, kind=dbgkind).ap()
    h2d = nc.dram_tensor("h2d", [S, D], BF16, kind=dbgkind).ap()
    c_zT = din("c_zT", [33, S]); c_ndel = din("c_ndel", [128, 8]); c_tn = din("c_tn", [128, 512])
    c_FT1 = din("c_FT1", [64, 128, 128], BF16); c_MI = din("c_MI", [128, 128, 32], BF16); c_G = din("c_G", [128, 3, 128], BF16)

    with ExitStack() as es:
        P = Prog(nc, es)
        ps = [es.enter_context(nc.psum_tensor(f"ps{i}", [128, 512], F32)) for i in range(8)]
        ident = es.enter_context(nc.sbuf_tensor("ident", [128, 128], BF16))
        P.op("pool", lambda e: e.memset(ident[:], 1.0), w=["ident"])
        P.op("pool", lambda e: e.affine_select(out=ident[:], in_=ident[:], pattern=[[-1, 128]], compare_op=ALU.is_equal,
                                               fill=0.0, base=0, channel_multiplier=1), r=["ident"], w=["ident"])

        identf = es.enter_context(nc.sbuf_tensor("identf", [128, 128], F32))
        P.op("pool", lambda e: e.memset(identf[:], 1.0), w=["identf"])
        P.op("pool", lambda e: e.affine_select(out=identf[:], in_=identf[:], pattern=[[-1, 128]], compare_op=ALU.is_equal,
                                               fill=0.0, base=0, channel_multiplier=1), r=["identf"], w=["identf"])
        lg = es.enter_context(nc.sbuf_tensor("lg", [128, 32, 16], F32))
        idxI = es.enter_context(nc.sbuf_tensor("idxI", [128, 16, 4], I32))
        valT = es.enter_context(nc.sbuf_tensor("valT", [128, 16, 4], F32))
        with ExitStack() as p1:
            def sb(name, shape, dt):
                return p1.enter_context(nc.sbuf_tensor(name, shape, dt))
            hT = sb("hT", [128, 8, S], BF16)
            gB = sb("gB", [128, D], F32)
            xt = [sb(f"xt{i}", [128, D], F32) for i in range(2)]
            sq = sb("sq", [128, D], F32)
            hb = [sb(f"hb{i}", [128, D], BF16) for i in range(2)]
            ss = sb("ss", [128, 32], F32)
            rstd = sb("rstd", [128, 32], F32)
            caw = sb("caw", [128, 4, 8], F32)
            cab = sb("cab", [128, 8], F32)
            cbw = sb("cbw", [128, 3, 24], F32)
            cbb = sb("cbb", [128, 24], F32)
            wt = [sb(f"wt{i}", [128, 8, 512], BF16) for i in range(2)]
            pb = [sb(f"pb{i}", [128, S], F32) for i in range(2)]
            yb = [sb(f"yb{i}", [128, S], F32) for i in range(2)]

            P.dma("sp", "c0", lambda e: e.dma_start(out=gB[:], in_=g_mix.partition_broadcast(128)), w=["gB"])
            for kk_ in range(4):
                P.dma("sp", "c1", lambda e, kk_=kk_: e.dma_start(out=caw[:, kk_, :], in_=conv_a_w[kk_:kk_ + 1, :].rearrange("o (g c) -> c (o g)", c=128),
                                                    allow_slow_non_contiguous=True), w=["caw"])
            P.dma("sp", "c2", lambda e: e.dma_start(out=cab[:], in_=conv_a_b.rearrange("o (g c) -> c (o g)", c=128),
                                                    allow_slow_non_contiguous=True), w=["cab"])
            for kk_ in range(3):
                P.dma("sp", "c3", lambda e, kk_=kk_: e.dma_start(out=cbw[:, kk_, :], in_=conv_b_w[kk_:kk_ + 1, :].rearrange("o (g c) -> c (o g)", c=128),
                                                    allow_slow_non_contiguous=True), w=["cbw"])
            P.dma("sp", "c4", lambda e: e.dma_start(out=cbb[:], in_=conv_b_b.rearrange("o (g c) -> c (o g)", c=128),
                                                    allow_slow_non_contiguous=True), w=["cbb"])
            P.op("dve", lambda e: e.memset(ss[:], 0.0), w=["ss"])
            for tt in range(32):
                b = tt % 2
                P.dma("sp", f"x{b}", lambda e, b=b, tt=tt: e.dma_start(out=xt[b][:], in_=x[tt * 128:(tt + 1) * 128, :]), w=[f"xt{b}"])
                P.op("act", lambda e, b=b, tt=tt: e.activation(out=sq[:], in_=xt[b][:], func=AF.Square, accum_out=ss[:, tt:tt + 1]),
                     r=[f"xt{b}", "ss"], w=["sq", f"ss{tt}"])
                P.op("act", lambda e, tt=tt: e.activation(out=rstd[:, tt:tt + 1], in_=ss[:, tt:tt + 1], func=AF.Sqrt, bias=EPS, scale=1.0 / D),
                     r=[f"ss{tt}"], w=[f"rs{tt}"])
                P.op("dve", lambda e, tt=tt: e.reciprocal(out=rstd[:, tt:tt + 1], in_=rstd[:, tt:tt + 1]), r=[f"rs{tt}"], w=[f"rs{tt}"])
                P.op("dve", lambda e, b=b, tt=tt: e.scalar_tensor_tensor(out=hb[b][:], in0=xt[b][:], scalar=rstd[:, tt:tt + 1], in1=gB[:],
                                                                         op0=ALU.mult, op1=ALU.mult),
                     r=[f"xt{b}", f"rs{tt}", "gB"], w=[f"hb{b}"])
                pst = ps[b][:].bitcast(BF16)
                for dh in range(8):
                    P.op("pe", lambda e, b=b, dh=dh, pst=pst: e.transpose(pst[:, dh * 128:(dh + 1) * 128], hb[b][:, dh * 128:(dh + 1) * 128], ident[:]),
                         r=[f"hb{b}", "ident"], w=[f"ps{b}"])
                P.op("act", lambda e, tt=tt, b=b: e.activation(out=hT[:].bitcast(F32)[:, :, tt * 64:(tt + 1) * 64],
                                                                   in_=ps[b][:].rearrange("p (a t) -> p a t", a=8), func=AF.Identity),
                     r=[f"ps{b}"], w=["hT"])
            if upto >= 1:
                def colkind(cg):
                    if cg < 8:
                        return "ax"
                    if cg < 16:
                        return "ag"
                    if cg < 40:
                        return "hy"
                    return "gt"
                ev = 0
                for wl in range(14):
                    wb_ = wl % 2
                    P.dma("pool", f"w{wb_}", lambda e, wl=wl, wb_=wb_: e.dma_start(
                        out=wt[wb_][:], in_=w_in[:, wl * 512:(wl + 1) * 512].rearrange("(a p) c -> p a c", p=128)), w=[f"wt{wb_}"])
                    for sub in range(4):
                        cg = wl * 4 + sub
                        kind = colkind(cg)
                        pbi = cg % 2
                        func = {"ax": AF.Identity, "ag": AF.Gelu, "hy": AF.Identity, "gt": AF.Sigmoid}[kind]
                        for tc_ in range(8):
                            bank = 2 + (ev % 6)
                            ev += 1
                            for kk in range(8):
                                P.op("pe", lambda e, bank=bank, wb_=wb_, sub=sub, kk=kk, tc_=tc_: e.matmul(
                                    ps[bank][:], lhsT=wt[wb_][:, kk, sub * 128:(sub + 1) * 128], rhs=hT[:, kk, tc_ * 512:(tc_ + 1) * 512],
                                    start=(kk == 0), stop=(kk == 7)), r=[f"wt{wb_}", "hT"], w=[f"ps{bank}"])
                            P.op("act", lambda e, bank=bank, pbi=pbi, tc_=tc_, func=func: e.activation(
                                out=pb[pbi][:, tc_ * 512:(tc_ + 1) * 512], in_=ps[bank][:], func=func), r=[f"ps{bank}"], w=[f"pb{pbi}"])
                        src = pb[pbi]
                        if kind in ("ax", "hy"):
                            yy = yb[pbi]
                            if kind == "ax":
                                g = cg
                                wts = [caw[:, j, g:g + 1] for j in range(4)]
                                bias = cab[:, g:g + 1]
                                sh = [-2, -1, 0, 1]
                            else:
                                g = cg - 16
                                wts = [cbw[:, j, g:g + 1] for j in range(3)]
                                bias = cbb[:, g:g + 1]
                                sh = [-1, 0, 1]
                            ci = sh.index(0)
                            eng = "dve"
                            P.op(eng, lambda e, yy=yy, src=src, wts=wts, bias=bias, ci=ci: e.tensor_scalar(
                                out=yy[:], in0=src[:], scalar1=wts[ci], scalar2=bias, op0=ALU.mult, op1=ALU.add),
                                r=[f"pb{pbi}", "caw", "cab", "cbw", "cbb"], w=[f"yb{pbi}"])
                            for j, s_ in enumerate(sh):
                                if s_ == 0:
                                    continue
                                if s_ < 0:
                                    o_sl = slice(-s_, S)
                                    i_sl = slice(0, S + s_)
                                else:
                                    o_sl = slice(0, S - s_)
                                    i_sl = slice(s_, S)
                                P.op(eng, lambda e, yy=yy, src=src, wts=wts, j=j, o_sl=o_sl, i_sl=i_sl: e.scalar_tensor_tensor(
                                    out=yy[:, o_sl], in0=src[:, i_sl], scalar=wts[j], in1=yy[:, o_sl], op0=ALU.mult, op1=ALU.add),
                                    r=[f"pb{pbi}", f"yb{pbi}"], w=[f"yb{pbi}"])
                            P.dma("sp", f"poy{pbi}", lambda e, yy=yy, cg=cg: e.dma_start(out=projT[cg * 128:(cg + 1) * 128, :], in_=yy[:]),
                                  r=[f"yb{pbi}"], w=[f"projT{cg}"])
                        else:
                            P.dma("sp", f"pop{pbi}", lambda e, src=src, cg=cg: e.dma_start(out=projT[cg * 128:(cg + 1) * 128, :], in_=src[:]),
                                  r=[f"pb{pbi}"], w=[f"projT{cg}"])
        P.barrier()
        evc = [0]

        def evac_eng():
            evc[0] += 1
            return "act" if evc[0] % 4 == 2 else "dve"

        def copy_op(eng, out_ap, in_ap, r, w):
            if eng == "act":
                P.op("act", lambda e: e.activation(out=out_ap, in_=in_ap, func=AF.Identity), r=r, w=w)
            else:
                P.op(eng, lambda e: e.tensor_copy(out=out_ap, in_=in_ap), r=r, w=w)

        def bk(name, lo, hi):
            return [f"{name}_{i}" for i in range(lo // 512, (hi - 1) // 512 + 1)]

        def allk(name):
            return [f"{name}_{i}" for i in range(32)]
        bankc = [0]

        def nbank():
            bankc[0] += 1
            return bankc[0] % 8

        def phase2():
          with ExitStack() as p2:
            def sb(name, shape, dt):
                return p2.enter_context(nc.sbuf_tensor(name, shape, dt))
            lam = sb("lam", [128, 2, 8], F32)
            lamc = sb("lamc", [128, 2, 8], F32)
            lamc2 = sb("lamc2", [128, 2, 8], F32)
            br = sb("br", [128, 2, 8], F32)
            bi = sb("bi", [128, 2, 8], F32)
            bd = sb("bd", [128, 32, 128], BF16)
            xa = sb("xa", [128, S], F32)
            gg = sb("gg", [128, S], F32)
            xab = sb("xab", [128, S], BF16)
            rr_ = [sb(f"rr{i}", [128, S], F32) for i in range(2)]
            ig_ = [sb(f"ig{i}", [128, S], F32) for i in range(2)]
            aa_ = [sb(f"aa{i}", [128, S], F32) for i in range(2)]
            bx = sb("bx", [128, S], F32)
            hs = [sb(f"hs{i}", [128, S], F32) for i in range(2)]
            mo = sb("mo", [128, S], BF16)
            for d in range(2):
                for dst, srcd, kn in ((lam, lru_lambda, "lam"), (br, lru_b_r, "br"), (bi, lru_b_i, "bi")):
                    P.dma("sp", "c" + kn, lambda e, dst=dst, srcd=srcd, d=d: e.dma_start(
                        out=dst[:, d, :], in_=srcd[d:d + 1, :].rearrange("o (g c) -> c (o g)", c=128), allow_slow_non_contiguous=True), w=[kn])
            P.op("pool", lambda e: e.memset(bd[:], 0.0), w=["bd"])
            nb_ = 0
            for g in range(8):
                for d in range(2):
                    for kind, wsrc in enumerate((lru_w_r, lru_w_i)):
                        for hl in range(2):
                            nb_ += 1
                            P.dma("pool", f"bdl{nb_ % 4}", lambda e, g=g, d=d, kind=kind, wsrc=wsrc, hl=hl: e.dma_start(
                                out=bd[hl * 64:(hl + 1) * 64, g * 4 + d * 2 + kind, hl * 64:(hl + 1) * 64], in_=wsrc[d, 2 * g + hl, :, :]), r=["bd"], w=[f"bdx{nb_}"])
            bdkeys = [f"bdx{i}" for i in range(1, nb_ + 1)]
            P.op("act", lambda e: e.activation(out=lamc[:], in_=lam[:], func=AF.Exp, scale=-1.0), r=["lam"], w=["lamc"])
            P.op("act", lambda e: e.activation(out=lamc[:], in_=lamc[:], func=AF.Ln, bias=1.0, scale=1.0), r=["lamc"], w=["lamc"])
            P.op("dve", lambda e: e.tensor_scalar(out=lamc2[:], in0=lamc[:], scalar1=-16.0, scalar2=None, op0=ALU.mult), r=["lamc"], w=["lamc2"])
            P.op("dve", lambda e: e.tensor_scalar(out=lamc[:], in0=lamc[:], scalar1=-8.0, scalar2=None, op0=ALU.mult), r=["lamc", "lamc2"], w=["lamc"])
            first = True
            for g in range(8):
                P.dma("sp", "xa", lambda e, g=g: e.dma_start(out=xa[:], in_=projT[g * 128:(g + 1) * 128, :]), w=["xa"])
                P.dma("sp", "gg", lambda e, g=g: e.dma_start(out=gg[:], in_=projT[(8 + g) * 128:(9 + g) * 128, :]), w=["gg"])
                P.op("act", lambda e: e.activation(out=xab[:], in_=xa[:], func=AF.Identity), r=["xa"], w=["xab"])
                for d in range(2):
                    rr = rr_[d]
                    ig = ig_[d]
                    aa = aa_[d]
                    krr, kig, kaa = f"rr{d}", f"ig{d}", f"aa{d}"
                    for kind, dst, bias, dk in ((1, ig, bi, kig), (0, rr, br, krr)):
                        for tc_ in range(8):
                            bank = nbank()
                            P.op("pe", lambda e, bank=bank, g=g, d=d, kind=kind, tc_=tc_: e.matmul(
                                ps[bank][:], lhsT=bd[:, g * 4 + d * 2 + kind, :], rhs=xab[:, tc_ * 512:(tc_ + 1) * 512], start=True, stop=True),
                                r=["xab"] + (bdkeys if first else []), w=[f"ps{bank}"])
                            first = False
                            P.op("act", lambda e, bank=bank, dst=dst, bias=bias, d=d, g=g, tc_=tc_: e.activation(
                                out=dst[:, tc_ * 512:(tc_ + 1) * 512], in_=ps[bank][:], func=AF.Sigmoid, bias=bias[:, d, g:g + 1]),
                                r=[f"ps{bank}", "br", "bi"], w=[dk])
                    P.op("dve", lambda e, ig=ig: e.tensor_tensor(out=bx[:], in0=ig[:], in1=xa[:], op=ALU.mult), r=[kig, "xa"], w=["bx"])
                    P.op("act", lambda e, d=d, g=g, rr=rr, aa=aa: e.activation(out=aa[:], in_=rr[:], func=AF.Exp, scale=lamc[:, d, g:g + 1]), r=[krr, "lamc"], w=[kaa])
                    P.op("act", lambda e, d=d, g=g, rr=rr: e.activation(out=rr[:], in_=rr[:], func=AF.Exp, scale=lamc2[:, d, g:g + 1]), r=[krr, "lamc2"], w=[krr])
                    P.op("dve", lambda e, rr=rr: e.tensor_scalar(out=rr[:], in0=rr[:], scalar1=1.0, scalar2=-1.0, op0=ALU.min, op1=ALU.mult), r=[krr], w=[krr])
                    P.op("act", lambda e, rr=rr: e.activation(out=rr[:], in_=rr[:], func=AF.Sqrt, bias=1.0, scale=1.0), r=[krr], w=[krr])
                    st_ = 0 if d == 0 else S - 1
                    P.op("dve", lambda e, st_=st_, rr=rr: e.memset(rr[:, st_:st_ + 1], 1.0), r=[krr], w=[krr])
                    P.op("dve", lambda e, rr=rr: e.tensor_tensor(out=bx[:], in0=bx[:], in1=rr[:], op=ALU.mult), r=["bx", krr], w=["bx"])
                    if d == 0:
                        P.op("dve", lambda e, aa=aa: e.tensor_tensor_scan(out=hs[0][:], data0=aa[:], data1=bx[:], initial=0.0, op0=ALU.mult, op1=ALU.add),
                             r=[kaa, "bx"], w=["hs0"])
                    else:
                        P.op("dve", lambda e, aa=aa: e.tensor_tensor_scan(out=revap(hs[1][:], S), data0=revap(aa[:], S), data1=revap(bx[:], S), initial=0.0,
                                                                   op0=ALU.mult, op1=ALU.add), r=[kaa, "bx"], w=["hs1"])
                P.op("dve", lambda e: e.tensor_tensor(out=hs[0][:], in0=hs[0][:], in1=hs[1][:], op=ALU.add), r=["hs0", "hs1"], w=["hs0"])
                P.op("dve", lambda e: e.tensor_tensor(out=mo[:], in0=hs[0][:], in1=gg[:], op=ALU.mult), r=["hs0", "gg"], w=["mo"])
                P.dma("sp", "mo", lambda e, g=g: e.dma_start(out=maT[g * 128:(g + 1) * 128, :], in_=mo[:]), r=["mo"], w=[f"maT{g}"])
          P.barrier()

        if upto >= 2:
            phase2()

        def fft_fwd(srcfn, nhi, W0, W1, ft, G, FT1d, ftstream, srckey):
            V = W0[:].rearrange("p (l c) -> p l c", c=128)
            Bv = W1[:].rearrange("p (l c) -> p l c", c=128)
            for j in range(16):
                bank = nbank()
                pst = ps[bank][:].bitcast(BF16)
                for q in range(8):
                    tl = j * 8 + q
                    P.op("pe", lambda e, pst=pst, q=q, tl=tl: e.transpose(pst[0:nhi, q * 128:(q + 1) * 128], srcfn(tl), ident[:]),
                         r=[srckey, "ident"], w=[f"ps{bank}"])
                copy_op(evac_eng(), W0[:].bitcast(F32)[0:nhi, j * 512:(j + 1) * 512], ps[bank][0:nhi, :], [f"ps{bank}"], bk("W0", j * 1024, (j + 1) * 1024))
            for s_ in range(8):
                fb = s_ % 2
                P.dma("sp", f"{ftstream}{fb}", lambda e, fb=fb, s_=s_: e.dma_start(out=ft[fb][0:64, :, :], in_=FT1d[:, s_ * 16:(s_ + 1) * 16, :]), w=[f"ft{fb}"])
                for jj in range(4):
                    j = s_ * 4 + jj
                    bank = nbank()
                    for q in range(4):
                        tl = j * 4 + q
                        P.op("pe", lambda e, bank=bank, q=q, tl=tl, fb=fb: e.matmul(
                            ps[bank][:, q * 128:(q + 1) * 128], lhsT=ft[fb][0:nhi, tl % 16, :], rhs=V[0:nhi, tl, :], start=True, stop=True),
                            r=[f"ft{fb}"] + bk("W0", tl * 128, tl * 128 + 128), w=[f"ps{bank}"])
                    copy_op(evac_eng(), Bv[:, j * 4:(j + 1) * 4, :], ps[bank][:].rearrange("p (a c) -> p a c", a=4), [f"ps{bank}"], bk("W1", j * 512, (j + 1) * 512))
            Cv = W0[:].rearrange("p (c f) -> p c f", f=128)
            for j in range(16):
                bank = nbank()
                pst = ps[bank][:].bitcast(BF16)
                for q in range(8):
                    c_ = j * 8 + q
                    P.op("pe", lambda e, pst=pst, q=q, c_=c_: e.transpose(pst[:, q * 128:(q + 1) * 128], Bv[:, :, c_], ident[:]),
                         r=allk("W1") + ["ident"], w=[f"ps{bank}"])
                copy_op(evac_eng(), W0[:].bitcast(F32)[:, j * 512:(j + 1) * 512], ps[bank][:], [f"ps{bank}"], bk("W0", j * 1024, (j + 1) * 1024))
            Xre = W1[:, 0:8192].rearrange("p (c f) -> p c f", f=64)
            Xim = W1[:, 8192:16384].rearrange("p (c f) -> p c f", f=64)
            for j in range(16):
                rre = Cv[:, j * 8:(j + 1) * 8, 0:64]
                rim = Cv[:, j * 8:(j + 1) * 8, 64:128]
                rk = bk("W0", j * 1024, (j + 1) * 1024) + ["G"]
                b1 = nbank()
                o1 = ps[b1][:].rearrange("p (a f) -> p a f", a=8)
                P.op("pe", lambda e, o1=o1, rre=rre: e.matmul(o1, lhsT=G[:, 0, :], rhs=rre, start=True, stop=False), r=rk, w=[f"ps{b1}"])
                P.op("pe", lambda e, o1=o1, rim=rim: e.matmul(o1, lhsT=G[:, 2, :], rhs=rim, start=False, stop=True), r=rk, w=[f"ps{b1}"])
                copy_op(evac_eng(), Xre[:, j * 8:(j + 1) * 8, :], o1, [f"ps{b1}"], bk("W1", j * 512, (j + 1) * 512))
                b2 = nbank()
                o2 = ps[b2][:].rearrange("p (a f) -> p a f", a=8)
                P.op("pe", lambda e, o2=o2, rre=rre: e.matmul(o2, lhsT=G[:, 1, :], rhs=rre, start=True, stop=False), r=rk, w=[f"ps{b2}"])
                P.op("pe", lambda e, o2=o2, rim=rim: e.matmul(o2, lhsT=G[:, 0, :], rhs=rim, start=False, stop=True), r=rk, w=[f"ps{b2}"])
                copy_op(evac_eng(), Xim[:, j * 8:(j + 1) * 8, :], o2, [f"ps{b2}"], bk("W1", 8192 + j * 512, 8192 + (j + 1) * 512))

        MAGIC = 12582912.0
        TWO_PI = float(2 * np.pi)
        def phase3a():
          with ExitStack() as p3:
            def sb(name, shape, dt):
                return p3.enter_context(nc.sbuf_tensor(name, shape, dt))
            zT = sb("zT", [33, S], F32)
            w1s = sb("w1s", [33, 64], F32)
            w2s = sb("w2s", [64, 64], F32)
            b1s = sb("b1s", [64, 1], F32)
            b2s = sb("b2s", [64, 1], F32)
            fq = sb("fq", [64, 2], F32)
            H1T = sb("H1T", [64, S], F32)
            H2T = sb("H2T", [64, S], F32)
            w3s = sb("w3s", [64, 4096], BF16)
            H2Tb = sb("H2Tb", [64, S], BF16)
            G = sb("G", [128, 3, 128], BF16)
            ndel = sb("ndel", [128, 8], F32)
            nbias = sb("nbias", [128, 8, 8], F32)
            tn = sb("tn", [128, 512], F32)
            fbias = sb("fbias", [128, 2, 8], F32)
            ft = [sb(f"ft{i}", [64, 16, 128], BF16) for i in range(2)]
            W0 = sb("W0", [128, 16384], BF16)
            W1 = sb("W1", [128, 16384], BF16)
            kc = sb("kc", [128, 8192], BF16)
            kc1 = sb("kc1", [128, 8192], BF16)
            dec = [sb(f"dec{i}", [128, 512], F32) for i in range(2)]
            tA = sb("tA", [64, 512], F32)
            tB = sb("tB", [64, 512], F32)
            for dst, srcd, kn in ((zT, c_zT, "zT"), (w1s, filt_w1, "w1s"), (w2s, filt_w2, "w2s"), (b1s, filt_b1, "b1s"), (b2s, filt_b2, "b2s"),
                                  (fq, filt_freq, "fq"), (G, c_G, "G"), (ndel, c_ndel, "ndel"), (tn, c_tn, "tn")):
                P.dma("sp", "k" + kn, lambda e, dst=dst, srcd=srcd: e.dma_start(out=dst[:], in_=srcd), w=[kn])
            P.dma("pool", "kw3s", lambda e: e.dma_start(out=w3s[:], in_=filt_w3), w=["w3s"])
            for n in range(2):
                P.dma("sp", "kfb", lambda e, n=n: e.dma_start(out=fbias[:, n, :], in_=filt_bias[n:n + 1, :].rearrange("o (g c) -> c (o g)", c=128),
                                                            allow_slow_non_contiguous=True), w=["fbias"])
            for j in range(8):
                P.op("dve", lambda e, j=j: e.tensor_scalar(out=nbias[:, :, j], in0=ndel[:], scalar1=float(512 * j / (S - 1)), scalar2=None, op0=ALU.mult),
                     r=["ndel"], w=["nbias"])
            for layer, (wl_, bl_, kdim, srcT, dstT, fcol) in enumerate(((w1s, b1s, 33, zT, H1T, 0), (w2s, b2s, 64, H1T, H2T, 1))):
                for j in range(8):
                    bank = nbank()
                    P.op("pe", lambda e, bank=bank, wl_=wl_, kdim=kdim, srcT=srcT, j=j: e.matmul(
                        ps[bank][0:64, :], lhsT=wl_[0:kdim, :], rhs=srcT[0:kdim, j * 512:(j + 1) * 512], start=True, stop=True),
                        r=["w1s", "w2s", "zT", f"H{layer}"], w=[f"ps{bank}"])
                    P.op("dve", lambda e, bank=bank, bl_=bl_, fcol=fcol: e.tensor_scalar(out=tA[:], in0=ps[bank][0:64, :], scalar1=bl_[:, 0:1], scalar2=fq[:, fcol:fcol + 1],
                                                                                      op0=ALU.add, op1=ALU.mult), r=[f"ps{bank}", "b1s", "b2s", "fq"], w=["tA"])
                    P.op("dve", lambda e: e.tensor_scalar(out=tB[:], in0=tA[:], scalar1=1.0 / TWO_PI, scalar2=MAGIC, op0=ALU.mult, op1=ALU.add), r=["tA"], w=["tB"])
                    P.op("dve", lambda e: e.tensor_scalar(out=tB[:], in0=tB[:], scalar1=-MAGIC, scalar2=None, op0=ALU.add), r=["tB"], w=["tB"])
                    P.op("dve", lambda e: e.scalar_tensor_tensor(out=tA[:], in0=tB[:], scalar=-TWO_PI, in1=tA[:], op0=ALU.mult, op1=ALU.add), r=["tA", "tB"], w=["tA"])
                    P.op("act", lambda e, dstT=dstT, j=j: e.activation(out=dstT[:, j * 512:(j + 1) * 512], in_=tA[:], func=AF.Sin, scale=1.0 - 2e-6),
                         r=["tA"], w=[f"H{layer + 1}"])
            if debug:
                P.dma("sp", "h2d", lambda e: e.dma_start(out=H2dbg, in_=H2T[:]), r=["H2"], w=["H2dbg"])
            P.op("dve", lambda e: e.tensor_copy(out=H2Tb[:], in_=H2T[:]), r=["H2"], w=["H2b"])
            kcs = [kc, kc1]

            def gen_filter(idx):
                g, n = idx // 2, idx % 2
                kcb = kcs[idx % 2]
                fk = f"fsrc{idx % 2}"
                P.op("pool", lambda e: e.memset(kcb[:, 4096:4097], 0.0), r=[fk], w=[fk])
                for j in range(8):
                    db = j % 2
                    P.op("act", lambda e, db=db, j=j: e.activation(out=dec[db][:], in_=tn[:], func=AF.Exp, scale=ndel[:, g:g + 1], bias=nbias[:, g, j:j + 1]),
                         r=["tn", "ndel", "nbias"], w=[f"dec{db}"])
                    for dirn in range(2):
                        col0 = n * 2048 + dirn * 1024 + g * 128
                        bank = nbank()
                        P.op("pe", lambda e, bank=bank, col0=col0, j=j: e.matmul(ps[bank][:], lhsT=w3s[0:64, col0:col0 + 128], rhs=H2Tb[0:64, j * 512:(j + 1) * 512],
                                                                                 start=True, stop=True), r=["w3s", "H2b"], w=[f"ps{bank}"])
                        if dirn == 0:
                            P.op("dve", lambda e, bank=bank, db=db, j=j: e.tensor_tensor(out=kcb[:, j * 512:(j + 1) * 512], in0=ps[bank][:], in1=dec[db][:], op=ALU.mult),
                                 r=[f"ps{bank}", f"dec{db}"], w=[fk])
                        else:
                            lo = 1 if j == 0 else 0
                            nel = 512 - lo
                            p0 = 8192 - (j * 512 + 511)
                            P.op("dve", lambda e, bank=bank, db=db, lo=lo, nel=nel, p0=p0: e.tensor_tensor(
                                out=kcb[:, p0:p0 + nel], in0=revap(ps[bank][:, lo:512], nel), in1=revap(dec[db][:, lo:512], nel), op=ALU.mult),
                                r=[f"ps{bank}", f"dec{db}"], w=[fk])
                P.op("dve", lambda e: e.tensor_scalar(out=kcb[:, 0:1], in0=kcb[:, 0:1], scalar1=fbias[:, n, g:g + 1], scalar2=None, op0=ALU.add),
                     r=[fk, "fbias"], w=[fk])

            def fft_filter(idx):
                kcb = kcs[idx % 2]
                kv = kcb[:].rearrange("c (h l) -> c l h", l=128)
                fft_fwd(lambda tl, kv=kv: kv[:, tl, 0:64], 64, W0, W1, ft, G, c_FT1, "ftA", f"fsrc{idx % 2}")
                P.dma("sp", "ksp", lambda e: e.dma_start(out=Kspec[idx], in_=W1[:].rearrange("p (r x) -> p r x", r=2)),
                      r=allk("W1"), w=[f"Kspec{idx}"])
            gen_filter(0)
            for idx in range(16):
                if idx + 1 < 16:
                    gen_filter(idx + 1)
                fft_filter(idx)
          P.barrier()

        if upto >= 3:
            phase3a()

        def phase3b():
          with ExitStack() as p4:
            def sb(name, shape, dt):
                return p4.enter_context(nc.sbuf_tensor("b_" + name, shape, dt))
            G = sb("G", [128, 3, 128], BF16)
            MI = sb("MI", [128, 128, 32], BF16)
            ft = [sb(f"ft{i}", [64, 16, 128], BF16) for i in range(2)]
            W0 = sb("W0", [128, 16384], BF16)
            W1 = sb("W1", [128, 16384], BF16)
            Kc = [sb(f"Kc{i}", [128, 2, 2048], BF16) for i in range(2)]
            tmps = [sb(f"tm{i}", [128, 2048], BF16) for i in range(4)]
            ub = [sb(f"ub{i}", [128, S], BF16) for i in range(2)]
            gate = sb("gate", [128, S], F32)
            mo = sb("mo2", [128, S], BF16)
            P.dma("sp", "kG", lambda e: e.dma_start(out=G[:], in_=c_G), w=["G"])
            P.dma("sp", "kMI", lambda e: e.dma_start(out=MI[:], in_=c_MI), w=["MI"])
            kq = 0
            for g in range(8):
                P.dma("pool", "uld", lambda e, g=g: e.dma_start(out=ub[0][:], in_=projT[(16 + g) * 128:(17 + g) * 128, :]), w=["fsrc0"])
                for n in range(2):
                    usrc = ub[n]
                    udst = ub[1] if n == 0 else mo
                    udk = "fsrc1" if n == 0 else "mo2"
                    grow = (24 + g) if n == 0 else (32 + g)
                    P.dma("sp", "gld", lambda e, grow=grow: e.dma_start(out=gate[:], in_=projT[grow * 128:(grow + 1) * 128, :]), w=["gate"])
                    uv = usrc[:].rearrange("c (h l) -> c l h", l=128)
                    fft_fwd(lambda tl, uv=uv: uv[:, tl, 0:32], 32, W0, W1, ft, G, c_FT1, "ftB", f"fsrc{n}")
                    for q in range(4):
                        kb = kq % 2
                        kq += 1
                        P.dma("sp", f"kc{kb}", lambda e, kb=kb, g=g, n=n, q=q: e.dma_start(
                            out=Kc[kb][:], in_=Kspec[g * 2 + n][:, :, q * 2048:(q + 1) * 2048]), w=[f"Kc{kb}"])
                        xre = W1[:, q * 2048:(q + 1) * 2048]
                        xim = W1[:, 8192 + q * 2048:8192 + (q + 1) * 2048]
                        yre = W0[:, q * 2048:(q + 1) * 2048]
                        yim = W0[:, 8192 + q * 2048:8192 + (q + 1) * 2048]
                        kre = Kc[kb][:, 0, :]
                        kim = Kc[kb][:, 1, :]
                        xrk = bk("W1", q * 2048, (q + 1) * 2048)
                        xik = bk("W1", 8192 + q * 2048, 8192 + (q + 1) * 2048)
                        yrk = bk("W0", q * 2048, (q + 1) * 2048)
                        yik = bk("W0", 8192 + q * 2048, 8192 + (q + 1) * 2048)
                        P.op("dve", lambda e, xre=xre, kre=kre: e.tensor_tensor(out=tmps[0][:], in0=xre, in1=kre, op=ALU.mult), r=xrk + [f"Kc{kb}"], w=["tm0"])
                        P.op("dve", lambda e, xim=xim, kim=kim: e.tensor_tensor(out=tmps[1][:], in0=xim, in1=kim, op=ALU.mult), r=xik + [f"Kc{kb}"], w=["tm1"])
                        P.op("dve", lambda e, yre=yre: e.tensor_tensor(out=yre, in0=tmps[0][:], in1=tmps[1][:], op=ALU.subtract), r=["tm0", "tm1"], w=yrk)
                        P.op("dve", lambda e, xre=xre, kim=kim: e.tensor_tensor(out=tmps[2][:], in0=xre, in1=kim, op=ALU.mult), r=xrk + [f"Kc{kb}"], w=["tm2"])
                        P.op("dve", lambda e, xim=xim, kre=kre: e.tensor_tensor(out=tmps[3][:], in0=xim, in1=kre, op=ALU.mult), r=xik + [f"Kc{kb}"], w=["tm3"])
                        P.op("dve", lambda e, yim=yim: e.tensor_tensor(out=yim, in0=tmps[2][:], in1=tmps[3][:], op=ALU.add), r=["tm2", "tm3"], w=yik)
                    Yre = W0[:, 0:8192].rearrange("p (c f) -> p c f", f=64)
                    Yim = W0[:, 8192:16384].rearrange("p (c f) -> p c f", f=64)
                    Dv = W1[:].rearrange("p (c f) -> p c f", f=128)
                    for j in range(16):
                        rre = Yre[:, j * 8:(j + 1) * 8, :]
                        rim = Yim[:, j * 8:(j + 1) * 8, :]
                        rk = bk("W0", j * 512, (j + 1) * 512) + bk("W0", 8192 + j * 512, 8192 + (j + 1) * 512) + ["G"]
                        wk = bk("W1", j * 1024, (j + 1) * 1024)
                        b1 = nbank()
                        o1 = ps[b1][:].rearrange("p (a f) -> p a f", a=8)
                        P.op("pe", lambda e, o1=o1, rre=rre: e.matmul(o1, lhsT=G[:, 0, :], rhs=rre, start=True, stop=False), r=rk, w=[f"ps{b1}"])
                        P.op("pe", lambda e, o1=o1, rim=rim: e.matmul(o1, lhsT=G[:, 1, :], rhs=rim, start=False, stop=True), r=rk, w=[f"ps{b1}"])
                        copy_op(evac_eng(), Dv[:, j * 8:(j + 1) * 8, 0:64], o1, [f"ps{b1}"], wk)
                        b2 = nbank()
                        o2 = ps[b2][:].rearrange("p (a f) -> p a f", a=8)
                        P.op("pe", lambda e, o2=o2, rre=rre: e.matmul(o2, lhsT=G[:, 2, :], rhs=rre, start=True, stop=False), r=rk, w=[f"ps{b2}"])
                        P.op("pe", lambda e, o2=o2, rim=rim: e.matmul(o2, lhsT=G[:, 0, :], rhs=rim, start=False, stop=True), r=rk, w=[f"ps{b2}"])
                        copy_op(evac_eng(), Dv[:, j * 8:(j + 1) * 8, 64:128], o2, [f"ps{b2}"], wk)
                    Ev = W0[:].rearrange("p (c l) -> p l c", l=128)
                    EvT = W0[:].rearrange("p (l c) -> p c l", c=128)
                    for j in range(16):
                        bank = nbank()
                        pst = ps[bank][:].bitcast(BF16)
                        for q in range(8):
                            c_ = j * 8 + q
                            P.op("pe", lambda e, pst=pst, q=q, c_=c_: e.transpose(pst[:, q * 128:(q + 1) * 128], Dv[:, c_, :], ident[:]),
                                 r=bk("W1", c_ * 128, c_ * 128 + 128) + ["ident"], w=[f"ps{bank}"])
                        copy_op(evac_eng(), W0[:].bitcast(F32)[:, j * 512:(j + 1) * 512], ps[bank][:], [f"ps{bank}"], bk("W0", j * 1024, (j + 1) * 1024))
                    uo = udst[:].rearrange("c (h l) -> c h l", l=128)
                    gv = gate[:].rearrange("c (h l) -> c h l", l=128)
                    for j in range(8):
                        bank = nbank()
                        psv = ps[bank][:].rearrange("p (h q) -> p h q", q=16)
                        for q in range(16):
                            tl = j * 16 + q
                            P.op("pe", lambda e, psv=psv, q=q, tl=tl: e.matmul(psv[:, :, q], lhsT=Ev[:, tl, :], rhs=MI[:, tl, :], start=True, stop=True),
                                 r=allk("W0") + ["MI"], w=[f"ps{bank}"])
                        P.op("dve", lambda e, psv=psv, j=j, uo=uo, gv=gv: e.tensor_tensor(
                            out=uo[:, :, j * 16:(j + 1) * 16], in0=psv, in1=gv[:, :, j * 16:(j + 1) * 16], op=ALU.mult),
                            r=[f"ps{bank}", "gate"], w=[udk])
                P.dma("sp", "mo2", lambda e, g=g: e.dma_start(out=mbT[g * 128:(g + 1) * 128, :], in_=mo[:]), r=["mo2"], w=[f"mbT{g}"])
          P.barrier()
        if upto >= 4:
            phase3b()

        def bcast_last(t_ap, n):
            a = t_ap.ap
            return bass.AP(t_ap.tensor, t_ap.offset, [list(x) for x in a] + [[0, n]])

        def phase4():
          with ExitStack() as p5:
            def sb(name, shape, dt):
                return p5.enter_context(nc.sbuf_tensor("d_" + name, shape, dt))
            waT = sb("waT", [128, 8, 1024], BF16)
            wbT = sb("wbT", [128, 8, 1024], BF16)
            woT = sb("woT", [128, 8, 1024], BF16)
            mA = sb("mA", [128, 8, 512], BF16)
            mB = sb("mB", [128, 8, 512], BF16)
            gA = sb("gA", [128, 8, 512], F32)
            gBt = sb("gBt", [128, 8, 512], F32)
            t1 = sb("t1", [128, 512], F32)
            t2 = sb("t2", [128, 512], F32)
            mg = sb("mg", [128, 8, 512], BF16)
            xt = sb("xt", [128, 1024], F32)
            x1t = sb("x1t", [128, 1024], F32)
            sq = sb("sq", [128, 1024], F32)
            h2f = sb("h2f", [128, 1024], F32)
            h2b = sb("h2b", [128, 1024], BF16)
            h2T = sb("h2T", [128, 8, 128], F32)
            gF = sb("gF", [128, 1024], F32)
            wr = sb("wr", [128, 8, 16], F32)
            ss = sb("ss", [128, 32], F32)
            rstd = sb("rstd", [128, 32], F32)
            for dst, srcw, kn in ((waT, w_a_out, "waT"), (wbT, w_b_out, "wbT"), (woT, w_o, "woT")):
                P.dma("pool", "L" + kn, lambda e, dst=dst, srcw=srcw: e.dma_start(out=dst[:], in_=srcw.rearrange("(a p) d -> p a d", p=128)), w=[kn])
            P.dma("sp", "LgF", lambda e: e.dma_start(out=gF[:], in_=g_ffn.partition_broadcast(128)), w=["gF"])
            P.dma("sp", "Lwr", lambda e: e.dma_start(out=wr[:], in_=w_router.rearrange("(a p) e -> p a e", p=128)), w=["wr"])
            P.op("dve", lambda e: e.memset(ss[:], 0.0), w=["ss"])
            for tc_ in range(8):
                sl = slice(tc_ * 512, (tc_ + 1) * 512)
                P.dma("sp", "LmA", lambda e, sl=sl: e.dma_start(out=mA[:], in_=maT.rearrange("(a p) t -> p a t", p=128)[:, :, sl]), w=["mA"])
                P.dma("sp", "LmB", lambda e, sl=sl: e.dma_start(out=mB[:], in_=mbT.rearrange("(a p) t -> p a t", p=128)[:, :, sl]), w=["mB"])
                P.dma("sp", "LgA", lambda e, sl=sl: e.dma_start(out=gA[:], in_=projT[5120:6144, :].rearrange("(a p) t -> p a t", p=128)[:, :, sl]), w=["gA"])
                P.dma("sp", "LgB", lambda e, sl=sl: e.dma_start(out=gBt[:], in_=projT[6144:7168, :].rearrange("(a p) t -> p a t", p=128)[:, :, sl]), w=["gBt"])
                for dc in range(8):
                    ba = nbank()
                    for kk in range(8):
                        P.op("pe", lambda e, ba=ba, kk=kk, dc=dc: e.matmul(ps[ba][:], lhsT=waT[:, kk, dc * 128:(dc + 1) * 128], rhs=mA[:, kk, :], start=(kk == 0), stop=(kk == 7)),
                             r=["waT", "mA"], w=[f"ps{ba}"])
                    bb = nbank()
                    for kk in range(8):
                        P.op("pe", lambda e, bb=bb, kk=kk, dc=dc: e.matmul(ps[bb][:], lhsT=wbT[:, kk, dc * 128:(dc + 1) * 128], rhs=mB[:, kk, :], start=(kk == 0), stop=(kk == 7)),
                             r=["wbT", "mB"], w=[f"ps{bb}"])
                    P.op("dve", lambda e, ba=ba, dc=dc: e.tensor_tensor(out=t1[:], in0=ps[ba][:], in1=gA[:, dc, :], op=ALU.mult), r=[f"ps{ba}", "gA"], w=["t1"])
                    P.op("dve", lambda e, bb=bb, dc=dc: e.tensor_tensor(out=t2[:], in0=ps[bb][:], in1=gBt[:, dc, :], op=ALU.mult), r=[f"ps{bb}", "gBt"], w=["t2"])
                    P.op("dve", lambda e, dc=dc: e.tensor_tensor(out=mg[:, dc, :], in0=t1[:], in1=t2[:], op=ALU.add), r=["t1", "t2"], w=["mg"])
                for tq in range(4):
                    tt = tc_ * 4 + tq
                    P.dma("sp", "Lxt", lambda e, tt=tt: e.dma_start(out=xt[:], in_=x[tt * 128:(tt + 1) * 128, :]), w=["xt"])
                    for half in range(2):
                        bo = nbank()
                        for kk in range(8):
                            P.op("pe", lambda e, bo=bo, kk=kk, tq=tq, half=half: e.matmul(ps[bo][:], lhsT=mg[:, kk, tq * 128:(tq + 1) * 128], rhs=woT[:, kk, half * 512:(half + 1) * 512],
                                                                                         start=(kk == 0), stop=(kk == 7)), r=["mg", "woT"], w=[f"ps{bo}"])
                        P.op("dve", lambda e, bo=bo, half=half: e.tensor_tensor(out=x1t[:, half * 512:(half + 1) * 512], in0=ps[bo][:], in1=xt[:, half * 512:(half + 1) * 512], op=ALU.add),
                             r=[f"ps{bo}", "xt"], w=["x1t"])
                    P.dma("sp", "Sx1", lambda e, tt=tt: e.dma_start(out=x1d[tt * 128:(tt + 1) * 128, :], in_=x1t[:]), r=["x1t"], w=[f"x1d{tt}"])
                    P.op("act", lambda e, tt=tt: e.activation(out=sq[:], in_=x1t[:], func=AF.Square, accum_out=ss[:, tt:tt + 1]), r=["x1t", "ss"], w=["sq", f"ss{tt}"])
                    P.op("act", lambda e, tt=tt: e.activation(out=rstd[:, tt:tt + 1], in_=ss[:, tt:tt + 1], func=AF.Sqrt, bias=EPS, scale=1.0 / D), r=[f"ss{tt}"], w=[f"rs{tt}"])
                    P.op("dve", lambda e, tt=tt: e.reciprocal(out=rstd[:, tt:tt + 1], in_=rstd[:, tt:tt + 1]), r=[f"rs{tt}"], w=[f"rs{tt}"])
                    P.op("dve", lambda e, tt=tt: e.scalar_tensor_tensor(out=h2f[:], in0=x1t[:], scalar=rstd[:, tt:tt + 1], in1=gF[:], op0=ALU.mult, op1=ALU.mult),
                         r=["x1t", f"rs{tt}", "gF"], w=["h2f"])
                    P.op("act", lambda e: e.activation(out=h2b[:], in_=h2f[:], func=AF.Identity), r=["h2f"], w=["h2b"])
                    P.dma("sp", "Sh2", lambda e, tt=tt: e.dma_start(out=h2d[tt * 128:(tt + 1) * 128, :], in_=h2b[:]), r=["h2b"], w=[f"h2d{tt}"])
                    b0 = nbank()
                    b1_ = nbank()
                    for dh in range(8):
                        bx_ = b0 if dh < 4 else b1_
                        P.op("pe", lambda e, bx_=bx_, dh=dh: e.transpose(ps[bx_][:, (dh % 4) * 128:(dh % 4 + 1) * 128], h2f[:, dh * 128:(dh + 1) * 128], identf[:]),
                             r=["h2f", "identf"], w=[f"ps{bx_}"])
                    P.op("act", lambda e, b0=b0: e.activation(out=h2T[:, 0:4, :], in_=ps[b0][:].rearrange("p (a t) -> p a t", a=4), func=AF.Identity), r=[f"ps{b0}"], w=["h2Ta"])
                    P.op("act", lambda e, b1_=b1_: e.activation(out=h2T[:, 4:8, :], in_=ps[b1_][:].rearrange("p (a t) -> p a t", a=4), func=AF.Identity), r=[f"ps{b1_}"], w=["h2Tb"])
                    bl = nbank()
                    for kk in range(8):
                        P.op("pe", lambda e, bl=bl, kk=kk: e.matmul(ps[bl][:, 0:16], lhsT=h2T[:, kk, :], rhs=wr[:, kk, :], start=(kk == 0), stop=(kk == 7)),
                             r=["h2Ta", "h2Tb", "wr"], w=[f"ps{bl}"])
                    P.op("dve", lambda e, bl=bl, tt=tt: e.tensor_copy(out=lg[:, tt, :], in_=ps[bl][:, 0:16]), r=[f"ps{bl}"], w=["lg"])
          P.barrier()

        def phase5():
          with ExitStack() as p6:
            def sb(name, shape, dt):
                return p6.enter_context(nc.sbuf_tensor("e_" + name, shape, dt))
            mx = sb("mx", [128, 32], F32)
            sm = sb("sm", [128, 32], F32)
            aff = sb("aff", [128, 32, 16], F32)
            affE = sb("affE", [16, S], F32)
            junk = sb("junk", [16, S], F32)
            ones = sb("ones", [16, S], F32)
            csum = sb("csum", [16, S], F32)
            lo = sb("lo", [16, 1], F32)
            hi = sb("hi", [16, 1], F32)
            mid = sb("mid", [16, 1], F32)
            cnt = sb("cnt", [16, 1], F32)
            flag = sb("flag", [16, 1], F32)
            ta = sb("ta", [16, 1], F32)
            tb = sb("tb", [16, 1], F32)
            cT = sb("cT", [128, 32, 16], F32)
            Rall = sb("Rall", [128, 32, 16, 5], BF16)
            tokab = sb("tokab", [128, 32, 2], F32)
            rf = sb("rf", [128, 32, 16], F32)
            r1 = sb("r1", [128, 32, 16], F32)
            iota1 = sb("iota1", [128, 512], F32)
            Pt = [sb(f"Pt{i}", [128, 512], BF16) for i in range(4)]
            P.dma("sp", "Lio", lambda e: e.dma_start(out=iota1[:], in_=c_iota1), w=["iota1"])
            P.dma("sp", "Ltk", lambda e: e.dma_start(out=tokab[:], in_=c_tokab), w=["tokab"])
            P.op("dve", lambda e: e.tensor_reduce(out=mx[:], in_=lg[:], axis=mybir.AxisListType.X, op=ALU.max), r=["lg"], w=["mx"])
            P.op("dve", lambda e: e.tensor_tensor(out=aff[:], in0=lg[:], in1=bcast_last(mx[:], 16), op=ALU.subtract), r=["lg", "mx"], w=["aff"])
            P.op("act", lambda e: e.activation(out=aff[:], in_=aff[:], func=AF.Exp), r=["aff"], w=["aff"])
            P.op("dve", lambda e: e.tensor_reduce(out=sm[:], in_=aff[:], axis=mybir.AxisListType.X, op=ALU.add), r=["aff"], w=["sm"])
            P.op("dve", lambda e: e.reciprocal(out=sm[:], in_=sm[:]), r=["sm"], w=["sm"])
            P.op("dve", lambda e: e.tensor_tensor(out=aff[:], in0=aff[:], in1=bcast_last(sm[:], 16), op=ALU.mult), r=["aff", "sm"], w=["aff"])
            for j in range(8):
                bank = nbank()
                for q in range(4):
                    tt = j * 4 + q
                    P.op("pe", lambda e, bank=bank, q=q, tt=tt: e.transpose(ps[bank][0:16, q * 128:(q + 1) * 128], aff[:, tt, :], identf[:]), r=["aff", "identf"], w=[f"ps{bank}"])
                P.op("act", lambda e, bank=bank, j=j: e.activation(out=affE[:, j * 512:(j + 1) * 512], in_=ps[bank][0:16, :], func=AF.Identity), r=[f"ps{bank}"], w=["affE"])
            P.op("dve", lambda e: e.memset(lo[:], 0.0), w=["lo"])
            P.op("dve", lambda e: e.memset(hi[:], 1.0), w=["hi"])
            P.op("pool", lambda e: e.memset(ones[:], 1.0), w=["ones"])
            D_ = "dve"
            for it in range(32):
                P.op(D_, lambda e: e.tensor_tensor(out=mid[:], in0=lo[:], in1=hi[:], op=ALU.add), r=["lo", "hi"], w=["mid"])
                P.op(D_, lambda e: e.tensor_scalar(out=mid[:], in0=mid[:], scalar1=0.5, scalar2=None, op0=ALU.mult), r=["mid"], w=["mid"])
                P.op(D_, lambda e: e.memset(cnt[:], 0.0), r=["cnt"], w=["cnt"])
                P.op("act", lambda e: e.activation(out=junk[:], in_=affE[:], func=AF.Sign, bias=mid[:, 0:1], scale=-1.0, accum_out=cnt[:, 0:1]), r=["affE", "mid", "cnt"], w=["junk", "cnt"])
                P.op(D_, lambda e: e.tensor_scalar(out=flag[:], in0=cnt[:], scalar1=3072.5, scalar2=None, op0=ALU.is_lt), r=["cnt"], w=["flag"])
                P.op(D_, lambda e: e.tensor_tensor(out=ta[:], in0=mid[:], in1=flag[:], op=ALU.mult), r=["mid", "flag"], w=["ta"])
                P.op(D_, lambda e: e.tensor_tensor(out=lo[:], in0=lo[:], in1=ta[:], op=ALU.max), r=["lo", "ta"], w=["lo"])
                P.op(D_, lambda e: e.tensor_scalar(out=tb[:], in0=flag[:], scalar1=-1.0, scalar2=1.0, op0=ALU.mult, op1=ALU.add), r=["flag"], w=["tb"])
                P.op(D_, lambda e: e.tensor_tensor(out=tb[:], in0=tb[:], in1=mid[:], op=ALU.mult), r=["tb", "mid"], w=["tb"])
                P.op(D_, lambda e: e.scalar_tensor_tensor(out=tb[:], in0=flag[:], scalar=2.0, in1=tb[:], op0=ALU.mult, op1=ALU.add), r=["flag", "tb"], w=["tb"])
                P.op(D_, lambda e: e.tensor_tensor(out=hi[:], in0=hi[:], in1=tb[:], op=ALU.min), r=["hi", "tb"], w=["hi"])
            P.op(D_, lambda e: e.tensor_scalar(out=junk[:], in0=affE[:], scalar1=lo[:, 0:1], scalar2=None, op0=ALU.is_gt), r=["affE", "lo"], w=["junk"])
            P.op(D_, lambda e: e.tensor_tensor_scan(out=csum[:], data0=ones[:], data1=junk[:], initial=0.0, op0=ALU.mult, op1=ALU.add), r=["ones", "junk"], w=["csum"])
            P.op(D_, lambda e: e.tensor_tensor(out=csum[:], in0=csum[:], in1=junk[:], op=ALU.mult), r=["csum", "junk"], w=["csum"])
            bank = nbank()
            for tt in range(32):
                P.op("pe", lambda e, bank=bank, tt=tt: e.transpose(ps[bank][:, tt * 16:(tt + 1) * 16], csum[0:16, tt * 128:(tt + 1) * 128], identf[0:16, 0:16]),
                     r=["csum", "identf"], w=[f"ps{bank}"])
            cTi = sb("cTi", [128, 32, 16], I32)
            P.op("act", lambda e, bank=bank: e.activation(out=cT[:], in_=ps[bank][:].rearrange("p (t x) -> p t x", x=16), func=AF.Identity, bias=0.25), r=[f"ps{bank}"], w=["cT"])
            P.op("dve", lambda e: e.tensor_copy(out=cTi[:], in_=cT[:]), r=["cT"], w=["cTi"])
            P.op("dve", lambda e: e.tensor_copy(out=cT[:], in_=cTi[:]), r=["cTi"], w=["cT"])
            P.op("pool", lambda e: e.tensor_copy(out=Rall[:, :, :, 0], in_=bcast_last(tokab[:, :, 0], 16)), r=["tokab"], w=["Rall0"])
            P.op("pool", lambda e: e.tensor_copy(out=Rall[:, :, :, 1], in_=bcast_last(tokab[:, :, 1], 16)), r=["tokab"], w=["Rall0"])
            P.op("pool", lambda e: e.tensor_copy(out=Rall[:, :, :, 2], in_=aff[:]), r=["aff"], w=["Rall1"])
            P.op("pool", lambda e: e.tensor_copy(out=rf[:], in_=Rall[:, :, :, 2]), r=["Rall1"], w=["rf"])
            P.op("pool", lambda e: e.tensor_tensor(out=r1[:], in0=aff[:], in1=rf[:], op=ALU.subtract), r=["aff", "rf"], w=["r1"])
            P.op("pool", lambda e: e.tensor_copy(out=Rall[:, :, :, 3], in_=r1[:]), r=["r1"], w=["Rall1"])
            P.op("pool", lambda e: e.tensor_copy(out=rf[:], in_=Rall[:, :, :, 3]), r=["Rall1"], w=["rf"])
            P.op("pool", lambda e: e.tensor_tensor(out=r1[:], in0=r1[:], in1=rf[:], op=ALU.subtract), r=["r1", "rf"], w=["r1"])
            P.op("pool", lambda e: e.tensor_copy(out=Rall[:, :, :, 4], in_=r1[:]), r=["r1"], w=["Rall1"])
            bIs = [nbank() for _ in range(4)]
            psIs = [ps[b_][:, 0:80].rearrange("p (x five) -> p x five", x=16) for b_ in bIs]
            pc = 0
            for ex in range(16):
                for tt in range(32):
                    pb_ = pc % 4
                    pc += 1
                    P.op("dve", lambda e, pb_=pb_, tt=tt, ex=ex: e.tensor_scalar(out=Pt[pb_][:], in0=iota1[:], scalar1=cT[:, tt, ex:ex + 1], scalar2=None, op0=ALU.is_equal),
                         r=["iota1", "cT"], w=[f"Pt{pb_}"])
                    for ch in range(4):
                        P.op("pe", lambda e, pb_=pb_, tt=tt, ex=ex, ch=ch: e.matmul(psIs[ch][:, ex, :], lhsT=Pt[pb_][:, ch * 128:(ch + 1) * 128], rhs=Rall[:, tt, ex, :],
                                                                                 start=(tt == 0), stop=(tt == 31)), r=[f"Pt{pb_}", "Rall0", "Rall1"], w=[f"ps{bIs[ch]}"])
            idxF = sb("idxF", [128, 16, 4], F32)
            psS = sb("psS", [128, 4, 16, 5], F32)
            for ch in range(4):
                P.op("dve", lambda e, ch=ch: e.tensor_copy(out=psS[:, ch, :, :], in_=psIs[ch]), r=[f"ps{bIs[ch]}"], w=["psS"])
                P.op("dve", lambda e, ch=ch: e.scalar_tensor_tensor(out=idxF[:, :, ch], in0=psS[:, ch, :, 0], scalar=64.0, in1=psS[:, ch, :, 1], op0=ALU.mult, op1=ALU.add), r=["psS"], w=["idxF"])
                P.op("dve", lambda e, ch=ch: e.tensor_tensor(out=valT[:, :, ch], in0=psS[:, ch, :, 2], in1=psS[:, ch, :, 3], op=ALU.add), r=["psS"], w=["valT"])
                P.op("dve", lambda e, ch=ch: e.tensor_tensor(out=valT[:, :, ch], in0=valT[:, :, ch], in1=psS[:, ch, :, 4], op=ALU.add), r=["psS", "valT"], w=["valT"])
            P.op("dve", lambda e: e.tensor_scalar(out=idxF[:], in0=idxF[:], scalar1=0.25, scalar2=None, op0=ALU.add), r=["idxF"], w=["idxF"])
            P.op("dve", lambda e: e.tensor_copy(out=idxI[:], in_=idxF[:]), r=["idxF"], w=["idxI"])
          P.barrier()

        def phase6():
          with ExitStack() as p7:
            def sb(name, shape, dt):
                return p7.enter_context(nc.sbuf_tensor("f_" + name, shape, dt))
            wgu = [sb(f"wgu{i}", [128, 8, 512], BF16) for i in range(4)]
            wd = [sb(f"wd{i}", [128, 16, 1024], BF16) for i in range(2)]
            xe = sb("xe", [128, 4, 1024], BF16)
            xeT = sb("xeT", [128, 8, 512], BF16)
            actT = sb("actT", [128, 16, 512], BF16)
            sg = sb("sg", [128, 512], F32)
            ye = [sb(f"ye{i}", [128, 1024], F32) for i in range(2)]
            wq = 0
            yq = 0
            for ex in range(16):
                wdb = ex % 2
                for ch in range(4):
                    P.dma("pool", f"ga{ch}", lambda e, ex=ex, ch=ch: e.indirect_dma_start(
                        out=xe[:, ch, :], out_offset=None, in_=h2d[:, :], in_offset=bass.IndirectOffsetOnAxis(ap=idxI[:, ex, ch:ch + 1], axis=0)),
                        r=["idxI"], w=[f"xe{ch}"])
                for ch in range(4):
                    bank = nbank()
                    pst = ps[bank][:].bitcast(BF16)
                    for dh in range(8):
                        P.op("pe", lambda e, pst=pst, dh=dh, ch=ch: e.transpose(pst[:, dh * 128:(dh + 1) * 128], xe[:, ch, dh * 128:(dh + 1) * 128], ident[:]),
                             r=[f"xe{ch}", "ident"], w=[f"ps{bank}"])
                    copy_op(evac_eng(), xeT[:].bitcast(F32)[:, :, ch * 64:(ch + 1) * 64], ps[bank][:].rearrange("p (a s) -> p a s", a=8), [f"ps{bank}"], ["xeT"])
                P.dma("pool", f"Lwd{wdb}", lambda e, ex=ex, wdb=wdb: e.dma_start(out=wd[wdb][:], in_=w_down[ex].rearrange("(a p) d -> p a d", p=128)), w=[f"wd{wdb}"])
                for fc in range(4):
                    gbuf = wq % 4
                    ubuf = (wq + 1) % 4
                    wq += 2
                    P.dma("pool", f"Lw{gbuf}", lambda e, ex=ex, fc=fc, gbuf=gbuf: e.dma_start(
                        out=wgu[gbuf][:], in_=w_gate[ex][:, fc * 512:(fc + 1) * 512].rearrange("(a p) f -> p a f", p=128)), w=[f"wgu{gbuf}"])
                    P.dma("pool", f"Lw{ubuf}", lambda e, ex=ex, fc=fc, ubuf=ubuf: e.dma_start(
                        out=wgu[ubuf][:], in_=w_up[ex][:, fc * 512:(fc + 1) * 512].rearrange("(a p) f -> p a f", p=128)), w=[f"wgu{ubuf}"])
                    for sub in range(4):
                        fi = fc * 4 + sub
                        bg = nbank()
                        for kk in range(8):
                            P.op("pe", lambda e, bg=bg, kk=kk, gbuf=gbuf, sub=sub: e.matmul(ps[bg][:], lhsT=wgu[gbuf][:, kk, sub * 128:(sub + 1) * 128], rhs=xeT[:, kk, :],
                                                                                      start=(kk == 0), stop=(kk == 7)), r=[f"wgu{gbuf}", "xeT"], w=[f"ps{bg}"])
                        bu = nbank()
                        for kk in range(8):
                            P.op("pe", lambda e, bu=bu, kk=kk, ubuf=ubuf, sub=sub: e.matmul(ps[bu][:], lhsT=wgu[ubuf][:, kk, sub * 128:(sub + 1) * 128], rhs=xeT[:, kk, :],
                                                                                      start=(kk == 0), stop=(kk == 7)), r=[f"wgu{ubuf}", "xeT"], w=[f"ps{bu}"])
                        P.op("act", lambda e, bg=bg: e.activation(out=sg[:], in_=ps[bg][:], func=AF.Silu), r=[f"ps{bg}"], w=["sg"])
                        P.op("dve", lambda e, bu=bu, fi=fi: e.tensor_tensor(out=actT[:, fi, :], in0=ps[bu][:], in1=sg[:], op=ALU.mult), r=[f"ps{bu}", "sg"], w=["actT"])
                for ch in range(4):
                    yb_ = yq % 2
                    yq += 1
                    for half in range(2):
                        bo = nbank()
                        for fh in range(16):
                            P.op("pe", lambda e, bo=bo, fh=fh, ch=ch, half=half, wdb=wdb: e.matmul(ps[bo][:], lhsT=actT[:, fh, ch * 128:(ch + 1) * 128], rhs=wd[wdb][:, fh, half * 512:(half + 1) * 512],
                                                                                             start=(fh == 0), stop=(fh == 15)), r=["actT", f"wd{wdb}"], w=[f"ps{bo}"])
                        P.op("dve" if half == 0 else "act",
                             (lambda e, bo=bo, yb_=yb_, half=half, ex=ex, ch=ch: e.tensor_scalar(out=ye[yb_][:, half * 512:(half + 1) * 512], in0=ps[bo][:], scalar1=valT[:, ex, ch:ch + 1], scalar2=None, op0=ALU.mult))
                             if half == 0 else
                             (lambda e, bo=bo, yb_=yb_, half=half, ex=ex, ch=ch: e.activation(out=ye[yb_][:, half * 512:(half + 1) * 512], in_=ps[bo][:], func=AF.Identity, scale=valT[:, ex, ch:ch + 1])),
                             r=[f"ps{bo}", "valT"], w=[f"ye{yb_}"])
                    P.dma("pool", "scat", lambda e, yb_=yb_, ex=ex, ch=ch: e.indirect_dma_start(
                        out=x1d[:, :], out_offset=bass.IndirectOffsetOnAxis(ap=idxI[:, ex, ch:ch + 1], axis=0), in_=ye[yb_][:, :], in_offset=None, compute_op=ALU.add),
                        r=[f"ye{yb_}", "idxI"], w=["x1dall"])
          P.barrier()

        def phase7():
          with ExitStack() as p8:
            def sb(name, shape, dt):
                return p8.enter_context(nc.sbuf_tensor("g_" + name, shape, dt))
            xt = [sb(f"xt{i}", [128, 1024], F32) for i in range(2)]
            ot = [sb(f"ot{i}", [128, 1024], F32) for i in range(2)]
            sq = sb("sq", [128, 1024], F32)
            gF = sb("gF", [128, 1024], F32)
            ss = sb("ss", [128, 32], F32)
            rstd = sb("rstd", [128, 32], F32)
            P.dma("sp", "LgF", lambda e: e.dma_start(out=gF[:], in_=g_final.partition_broadcast(128)), w=["gF"])
            P.op("dve", lambda e: e.memset(ss[:], 0.0), w=["ss"])
            for tt in range(32):
                b = tt % 2
                P.dma("sp", f"Lx{b}", lambda e, b=b, tt=tt: e.dma_start(out=xt[b][:], in_=x1d[tt * 128:(tt + 1) * 128, :]), w=[f"xt{b}"])
                P.op("act", lambda e, b=b, tt=tt: e.activation(out=sq[:], in_=xt[b][:], func=AF.Square, accum_out=ss[:, tt:tt + 1]), r=[f"xt{b}", "ss"], w=["sq", f"ss{tt}"])
                P.op("act", lambda e, tt=tt: e.activation(out=rstd[:, tt:tt + 1], in_=ss[:, tt:tt + 1], func=AF.Sqrt, bias=EPS, scale=1.0 / D), r=[f"ss{tt}"], w=[f"rs{tt}"])
                P.op("dve", lambda e, tt=tt: e.reciprocal(out=rstd[:, tt:tt + 1], in_=rstd[:, tt:tt + 1]), r=[f"rs{tt}"], w=[f"rs{tt}"])
                P.op("dve", lambda e, b=b, tt=tt: e.scalar_tensor_tensor(out=ot[b][:], in0=xt[b][:], scalar=rstd[:, tt:tt + 1], in1=gF[:], op0=ALU.mult, op1=ALU.mult),
                     r=[f"xt{b}", f"rs{tt}", "gF"], w=[f"ot{b}"])
                P.dma("sp", f"So{b}", lambda e, b=b, tt=tt: e.dma_start(out=out[tt * 128:(tt + 1) * 128, :], in_=ot[b][:]), r=[f"ot{b}"], w=[f"out{tt}"])
          P.barrier()

        if upto >= 5:
            phase4()
        if upto >= 6:
            phase5()
        if upto >= 7:
            phase6()
        if upto >= 8:
            phase7()
        k.P = P
        P.emit()
    return nc


_NC_CACHE = {}


def _in_map(inputs, b, consts):
    m = {"x": np.ascontiguousarray(inputs["x"][b])}
    for n in ["g_mix", "w_in", "conv_a_w", "conv_a_b", "lru_w_r", "lru_b_r", "lru_w_i", "lru_b_i", "lru_lambda", "conv_b_w", "conv_b_b",
              "filt_w1", "filt_w2", "filt_w3", "filt_bias", "w_a_out", "w_b_out", "w_o", "g_ffn", "w_router", "w_gate", "w_up", "w_down"]:
        a = np.asarray(inputs[n])[0]
        if a.ndim == 1:
            a = a[None]
        m[n] = np.ascontiguousarray(a.astype(np.float32))
    m["g_final"] = np.ascontiguousarray(np.asarray(inputs["g_final"]).reshape(1, -1).astype(np.float32))
    m["filt_b1"] = np.ascontiguousarray(np.asarray(inputs["filt_b1"])[0].reshape(64, 1))
    m["filt_b2"] = np.ascontiguousarray(np.asarray(inputs["filt_b2"])[0].reshape(64, 1))
    m["filt_freq"] = np.ascontiguousarray(np.asarray(inputs["filt_freq"])[0].T)
    m.update(consts)
    return m


def kernel(**inputs):
    if "nc" not in _NC_CACHE:
        _NC_CACHE["nc"] = build(debug=False)
    nc = _NC_CACHE["nc"]
    consts = make_consts()
    in_maps = [_in_map(inputs, b, consts) for b in range(8)]
    res = run_bass_kernel_spmd(nc, in_maps, core_ids=list(range(8)))
    return np.stack([np.asarray(r["out"]) for r in res.results], axis=0).astype(np.float32)
```

```python
import math
import numpy as np
import ml_dtypes
from contextlib import ExitStack
import concourse.bass as bass
import concourse.mybir as mybir
from concourse.bass_utils import run_bass_kernel_spmd


ENGS = ("pe", "act", "dve", "pool", "sp")


class Prog:
    def __init__(self, nc, es):
        self.nc = nc
        self.es = es
        self.ops = {e: [] for e in ENGS}
        self.sems = {}
        self.keys = {}
        self.seen = {e: {} for e in ENGS}
        for e in ENGS:
            self._sem("E_" + e)

    def _sem(self, name):
        if name not in self.sems:
            self.sems[name] = [self.es.enter_context(self.nc.semaphore("s_" + name)), 0]
        return self.sems[name]

    def _record(self, eng, fn, r, w, signame, inc):
        need = {}

        def req(sv):
            if sv is None:
                return
            s, v = sv
            if eng == "pe" and s == "E_pe":
                return
            if need.get(s, 0) < v:
                need[s] = v
        for k in r:
            st = self.keys.get(k)
            if st:
                req(st[0])
        for k in w:
            st = self.keys.get(k)
            if st:
                req(st[0])
                for s, v in st[1].items():
                    req((s, v))
        if signame.startswith("D_") and signame in self.sems and self.sems[signame][1] > 0:
            req((signame, self.sems[signame][1]))
        waits = []
        seen = self.seen[eng]
        for s, v in need.items():
            if seen.get(s, 0) < v:
                seen[s] = v
                waits.append((self.sems[s][0], v))
        sem = self._sem(signame)
        sem[1] += inc
        val = sem[1]
        for k in r:
            st = self.keys.setdefault(k, [None, {}])
            st[1][signame] = val
        for k in w:
            self.keys[k] = [(signame, val), {}]
        self.ops[eng].append((waits, fn, sem[0], inc))

    def op(self, eng, fn, r=(), w=()):
        self._record(eng, fn, r, w, "E_" + eng, 1)

    def dma(self, eng, stream, fn, r=(), w=()):
        self._record(eng, fn, r, w, "D_" + stream, 16)

    def barrier(self):
        tot = {s: v[1] for s, v in self.sems.items() if v[1] > 0}
        for e in ENGS:
            waits = []
            seen = self.seen[e]
            for s, v in tot.items():
                if s == "E_" + e:
                    continue
                if seen.get(s, 0) < v:
                    seen[s] = v
                    waits.append((self.sems[s][0], v))
            if waits:
                self.ops[e].append((waits, None, None, 0))
        self.keys = {}

    def emit(self):
        nc = self.nc
        with nc.Block() as block:
            def run(name):
                def body(e):
                    for waits, fn, sem, inc in self.ops[name]:
                        for s, v in waits:
                            e.wait_ge(s, v)
                        if fn is not None:
                            fn(e).then_inc(sem, inc)
                return body
            block.sync(run("sp"))
            block.scalar(run("act"))
            block.vector(run("dve"))
            block.gpsimd(run("pool"))
            block.tensor(run("pe"))


BF=ml_dtypes.bfloat16
def make_consts(S=4096):
    L=S; N=2*S
    t=np.linspace(0.0,1.0,L,dtype=np.float32)[:,None]
    w=(np.float32(2.0*math.pi/L)*np.arange(L,dtype=np.float32))[:,None]
    f=np.linspace(1e-4,15,16,dtype=np.float32)[None,:]
    z=np.concatenate([t,np.cos(w*f),-np.sin(w*f)],axis=-1).astype(np.float32)
    c={}
    c["c_zT"]=np.ascontiguousarray(z.T)
    min_decay=math.log(1e-2)/1.5; max_decay=math.log(1e-2)/0.3
    deltas=np.abs(np.linspace(min_decay,max_decay,1024,dtype=np.float32))
    c["c_ndel"]=np.ascontiguousarray((-deltas).reshape(8,128).T).astype(np.float32)
    c["c_tn"]=np.tile(t[:512,0][None,:],(128,1)).astype(np.float32)
    thi=np.arange(64)[:,None,None]; tlo=np.arange(128)[None,:,None]; fa=np.arange(64)[None,None,:]
    ang=2*np.pi*((128*thi+tlo)*fa % N)/N
    c["c_FT1"]=np.concatenate([np.cos(ang),-np.sin(ang)],axis=2).astype(BF)
    a=np.arange(128)
    ang=2*np.pi*(np.outer(a,a)%128)/128
    c["c_G"]=np.stack([np.cos(ang),-np.sin(ang),np.sin(ang)],axis=1).astype(BF)
    fa=np.arange(64)[:,None,None]; tlo=np.arange(128)[None,:,None]; thi=np.arange(32)[None,None,:]
    ang=2*np.pi*((tlo+128*thi)*fa % N)/N
    c["c_MI"]=(np.concatenate([np.cos(ang),-np.sin(ang)],axis=0)/N).astype(BF)
    c["c_iota1"] = np.tile(np.arange(1, 513, dtype=np.float32)[None, :], (128, 1))
    tokv = (np.arange(32)[None, :] * 128 + np.arange(128)[:, None])
    c["c_tokab"] = np.stack([tokv // 64, tokv % 64], axis=-1).astype(np.float32)
    return c


F32 = mybir.dt.float32
BF16 = mybir.dt.bfloat16
I32 = mybir.dt.int32
ALU = mybir.AluOpType
AF = mybir.ActivationFunctionType

S = 4096
D = 1024
NCOL = 7168
EPS = 1e-6


def revap(ap, n):
    a = ap.ap
    return bass.AP(ap.tensor, ap.offset + (n - 1) * a[-1][0], [list(x) for x in a[:-1]] + [[-a[-1][0], n]])


class K:
    pass


def build(debug=False, upto=99):
    nc = bass.Bass("TRN2", target_bir_lowering=False)
    k = K()
    k.nc = nc
    dbgkind = "ExternalOutput" if debug else "Internal"

    def din(name, shape, dt=F32):
        return nc.dram_tensor(name, list(shape), dt, kind="ExternalInput").ap()

    x = din("x", [S, D])
    g_mix = din("g_mix", [1, D])
    w_in = din("w_in", [D, NCOL])
    conv_a_w = din("conv_a_w", [4, 1024])
    conv_a_b = din("conv_a_b", [1, 1024])
    lru_w_r = din("lru_w_r", [2, 16, 64, 64])
    lru_b_r = din("lru_b_r", [2, 1024])
    lru_w_i = din("lru_w_i", [2, 16, 64, 64])
    lru_b_i = din("lru_b_i", [2, 1024])
    lru_lambda = din("lru_lambda", [2, 1024])
    conv_b_w = din("conv_b_w", [3, 3072])
    conv_b_b = din("conv_b_b", [1, 3072])
    out = nc.dram_tensor("out", [S, D], F32, kind="ExternalOutput").ap()
    projT = nc.dram_tensor("projT", [NCOL, S], F32, kind=dbgkind).ap()
    maT = nc.dram_tensor("maT", [1024, S], BF16, kind=dbgkind).ap()
    mbT = nc.dram_tensor("mbT", [1024, S], BF16, kind=dbgkind).ap()
    Kspec = nc.dram_tensor("Kspec", [16, 128, 2, 128 * 64], BF16, kind=dbgkind).ap()
    H2dbg = nc.dram_tensor("H2dbg", [64, S], F32, kind=dbgkind).ap()
    filt_w1 = din("filt_w1", [33, 64]); filt_b1 = din("filt_b1", [64, 1]); filt_w2 = din("filt_w2", [64, 64]); filt_b2 = din("filt_b2", [64, 1])
    filt_w3 = din("filt_w3", [64, 4096]); filt_freq = din("filt_freq", [64, 2]); filt_bias = din("filt_bias", [2, 1024])
    w_a_out = din("w_a_out", [D, D]); w_b_out = din("w_b_out", [D, D]); w_o = din("w_o", [D, D])
    g_ffn = din("g_ffn", [1, D]); w_router = din("w_router", [D, 16]); g_final = din("g_final", [1, D])
    w_gate = din("w_gate", [16, D, 2048]); w_up = din("w_up", [16, D, 2048]); w_down = din("w_down", [16, 2048, D])
    c_iota1 = din("c_iota1", [128, 512]); c_tokab = din("c_tokab", [128, 32, 2])
    x1d = nc.dram_tensor("x1d", [S, D], F32, kind=dbgkind).ap()
    h2d = nc.dram_tensor("h2d", [S, D], BF16, kind=dbgkind).ap()
    c_zT = din("c_zT", [33, S]); c_ndel = din("c_ndel", [128, 8]); c_tn = din("c_tn", [128, 512])
    c_FT1 = din("c_FT1", [64, 128, 128], BF16); c_MI = din("c_MI", [128, 128, 32], BF16); c_G = din("c_G", [128, 3, 128], BF16)

    with ExitStack() as es:
        P = Prog(nc, es)
        ps = [es.enter_context(nc.psum_tensor(f"ps{i}", [128, 512], F32)) for i in range(8)]
        ident = es.enter_context(nc.sbuf_tensor("ident", [128, 128], BF16))
        P.op("pool", lambda e: e.memset(ident[:], 1.0), w=["ident"])
        P.op("pool", lambda e: e.affine_select(out=ident[:], in_=ident[:], pattern=[[-1, 128]], compare_op=ALU.is_equal,
                                               fill=0.0, base=0, channel_multiplier=1), r=["ident"], w=["ident"])

        identf = es.enter_context(nc.sbuf_tensor("identf", [128, 128], F32))
        P.op("pool", lambda e: e.memset(identf[:], 1.0), w=["identf"])
        P.op("pool", lambda e: e.affine_select(out=identf[:], in_=identf[:], pattern=[[-1, 128]], compare_op=ALU.is_equal,
                                               fill=0.0, base=0, channel_multiplier=1), r=["identf"], w=["identf"])
        lg = es.enter_context(nc.sbuf_tensor("lg", [128, 32, 16], F32))
        idxI = es.enter_context(nc.sbuf_tensor("idxI", [128, 16, 4], I32))
        valT = es.enter_context(nc.sbuf_tensor("valT", [128, 16, 4], F32))
        with ExitStack() as p1:
            def sb(name, shape, dt):
                return p1.enter_context(nc.sbuf_tensor(name, shape, dt))
            hT = sb("hT", [128, 8, S], BF16)
            gB = sb("gB", [128, D], F32)
            xt = [sb(f"xt{i}", [128, D], F32) for i in range(2)]
            sq = sb("sq", [128, D], F32)
            hb = [sb(f"hb{i}", [128, D], BF16) for i in range(2)]
            ss = sb("ss", [128, 32], F32)
            rstd = sb("rstd", [128, 32], F32)
            caw = sb("caw", [128, 4, 8], F32)
            cab = sb("cab", [128, 8], F32)
            cbw = sb("cbw", [128, 3, 24], F32)
            cbb = sb("cbb", [128, 24], F32)
            wt = [sb(f"wt{i}", [128, 8, 512], BF16) for i in range(2)]
            pb = [sb(f"pb{i}", [128, S], F32) for i in range(2)]
            yb = [sb(f"yb{i}", [128, S], F32) for i in range(2)]

            P.dma("sp", "c0", lambda e: e.dma_start(out=gB[:], in_=g_mix.partition_broadcast(128)), w=["gB"])
            for kk_ in range(4):
                P.dma("sp", "c1", lambda e, kk_=kk_: e.dma_start(out=caw[:, kk_, :], in_=conv_a_w[kk_:kk_ + 1, :].rearrange("o (g c) -> c (o g)", c=128),
                                                    allow_slow_non_contiguous=True), w=["caw"])
            P.dma("sp", "c2", lambda e: e.dma_start(out=cab[:], in_=conv_a_b.rearrange("o (g c) -> c (o g)", c=128),
                                                    allow_slow_non_contiguous=True), w=["cab"])
            for kk_ in range(3):
                P.dma("sp", "c3", lambda e, kk_=kk_: e.dma_start(out=cbw[:, kk_, :], in_=conv_b_w[kk_:kk_ + 1, :].rearrange("o (g c) -> c (o g)", c=128),
                                                    allow_slow_non_contiguous=True), w=["cbw"])
            P.dma("sp", "c4", lambda e: e.dma_start(out=cbb[:], in_=conv_b_b.rearrange("o (g c) -> c (o g)", c=128),
                                                    allow_slow_non_contiguous=True), w=["cbb"])
            P.op("dve", lambda e: e.memset(ss[:], 0.0), w=["ss"])
            for tt in range(32):
                b = tt % 2
                P.dma("sp", f"x{b}", lambda e, b=b, tt=tt: e.dma_start(out=xt[b][:], in_=x[tt * 128:(tt + 1) * 128, :]), w=[f"xt{b}"])
                P.op("act", lambda e, b=b, tt=tt: e.activation(out=sq[:], in_=xt[b][:], func=AF.Square, accum_out=ss[:, tt:tt + 1]),
                     r=[f"xt{b}", "ss"], w=["sq", f"ss{tt}"])
                P.op("act", lambda e, tt=tt: e.activation(out=rstd[:, tt:tt + 1], in_=ss[:, tt:tt + 1], func=AF.Sqrt, bias=EPS, scale=1.0 / D),
                     r=[f"ss{tt}"], w=[f"rs{tt}"])
                P.op("dve", lambda e, tt=tt: e.reciprocal(out=rstd[:, tt:tt + 1], in_=rstd[:, tt:tt + 1]), r=[f"rs{tt}"], w=[f"rs{tt}"])
                P.op("dve", lambda e, b=b, tt=tt: e.scalar_tensor_tensor(out=hb[b][:], in0=xt[b][:], scalar=rstd[:, tt:tt + 1], in1=gB[:],
                                                                         op0=ALU.mult, op1=ALU.mult),
                     r=[f"xt{b}", f"rs{tt}", "gB"], w=[f"hb{b}"])
                pst = ps[b][:].bitcast(BF16)
                for dh in range(8):
                    P.op("pe", lambda e, b=b, dh=dh, pst=pst: e.transpose(pst[:, dh * 128:(dh + 1) * 128], hb[b][:, dh * 128:(dh + 1) * 128], ident[:]),
                         r=[f"hb{b}", "ident"], w=[f"ps{b}"])
                P.op("act", lambda e, tt=tt, b=b: e.activation(out=hT[:].bitcast(F32)[:, :, tt * 64:(tt + 1) * 64],
                                                                   in_=ps[b][:].rearrange("p (a t) -> p a t", a=8), func=AF.Identity),
                     r=[f"ps{b}"], w=["hT"])
            if upto >= 1:
                def colkind(cg):
                    if cg < 8:
                        return "ax"
                    if cg < 16:
                        return "ag"
                    if cg < 40:
                        return "hy"
                    return "gt"
                ev = 0
                for wl in range(14):
                    wb_ = wl % 2
                    P.dma("pool", f"w{wb_}", lambda e, wl=wl, wb_=wb_: e.dma_start(
                        out=wt[wb_][:], in_=w_in[:, wl * 512:(wl + 1) * 512].rearrange("(a p) c -> p a c", p=128)), w=[f"wt{wb_}"])
                    for sub in range(4):
                        cg = wl * 4 + sub
                        kind = colkind(cg)
                        pbi = cg % 2
                        func = {"ax": AF.Identity, "ag": AF.Gelu, "hy": AF.Identity, "gt": AF.Sigmoid}[kind]
                        for tc_ in range(8):
                            bank = 2 + (ev % 6)
                            ev += 1
                            for kk in range(8):
                                P.op("pe", lambda e, bank=bank, wb_=wb_, sub=sub, kk=kk, tc_=tc_: e.matmul(
                                    ps[bank][:], lhsT=wt[wb_][:, kk, sub * 128:(sub + 1) * 128], rhs=hT[:, kk, tc_ * 512:(tc_ + 1) * 512],
                                    start=(kk == 0), stop=(kk == 7)), r=[f"wt{wb_}", "hT"], w=[f"ps{bank}"])
                            P.op("act", lambda e, bank=bank, pbi=pbi, tc_=tc_, func=func: e.activation(
                                out=pb[pbi][:, tc_ * 512:(tc_ + 1) * 512], in_=ps[bank][:], func=func), r=[f"ps{bank}"], w=[f"pb{pbi}"])
                        src = pb[pbi]
                        if kind in ("ax", "hy"):
                            yy = yb[pbi]
                            if kind == "ax":
                                g = cg
                                wts = [caw[:, j, g:g + 1] for j in range(4)]
                                bias = cab[:, g:g + 1]
                                sh = [-2, -1, 0, 1]
                            else:
                                g = cg - 16
                                wts = [cbw[:, j, g:g + 1] for j in range(3)]
                                bias = cbb[:, g:g + 1]
                                sh = [-1, 0, 1]
                            ci = sh.index(0)
                            eng = "dve"
                            P.op(eng, lambda e, yy=yy, src=src, wts=wts, bias=bias, ci=ci: e.tensor_scalar(
                                out=yy[:], in0=src[:], scalar1=wts[ci], scalar2=bias, op0=ALU.mult, op1=ALU.add),
                                r=[f"pb{pbi}", "caw", "cab", "cbw", "cbb"], w=[f"yb{pbi}"])
                            for j, s_ in enumerate(sh):
                                if s_ == 0:
                                    continue
                                if s_ < 0:
                                    o_sl = slice(-s_, S)
                                    i_sl = slice(0, S + s_)
                                else:
                                    o_sl = slice(0, S - s_)
                                    i_sl = slice(s_, S)
                                P.op(eng, lambda e, yy=yy, src=src, wts=wts, j=j, o_sl=o_sl, i_sl=i_sl: e.scalar_tensor_tensor(
                                    out=yy[:, o_sl], in0=src[:, i_sl], scalar=wts[j], in1=yy[:, o_sl], op0=ALU.mult, op1=ALU.add),
                                    r=[f"pb{pbi}", f"yb{pbi}"], w=[f"yb{pbi}"])
                            P.dma("sp", f"poy{pbi}", lambda e, yy=yy, cg=cg: e.dma_start(out=projT[cg * 128:(cg + 1) * 128, :], in_=yy[:]),
                                  r=[f"yb{pbi}"], w=[f"projT{cg}"])
                        else:
                            P.dma("sp", f"pop{pbi}", lambda e, src=src, cg=cg: e.dma_start(out=projT[cg * 128:(cg + 1) * 128, :], in_=src[:]),
                                  r=[f"pb{pbi}"], w=[f"projT{cg}"])
        P.barrier()
        evc = [0]

        def evac_eng():
            evc[0] += 1
            return "act" if evc[0] % 7 in (2, 5) else "dve"

        def copy_op(eng, out_ap, in_ap, r, w):
            if eng == "act":
                P.op("act", lambda e: e.activation(out=out_ap, in_=in_ap, func=AF.Identity), r=r, w=w)
            else:
                P.op(eng, lambda e: e.tensor_copy(out=out_ap, in_=in_ap), r=r, w=w)

        def bk(name, lo, hi):
            return [f"{name}_{i}" for i in range(lo // 512, (hi - 1) // 512 + 1)]

        def allk(name):
            return [f"{name}_{i}" for i in range(32)]
        bankc = [0]

        def nbank():
            bankc[0] += 1
            return bankc[0] % 8

        def phase2():
          with ExitStack() as p2:
            def sb(name, shape, dt):
                return p2.enter_context(nc.sbuf_tensor(name, shape, dt))
            lam = sb("lam", [128, 2, 8], F32)
            lamc = sb("lamc", [128, 2, 8], F32)
            lamc2 = sb("lamc2", [128, 2, 8], F32)
            br = sb("br", [128, 2, 8], F32)
            bi = sb("bi", [128, 2, 8], F32)
            bd = sb("bd", [128, 32, 128], BF16)
            xa = sb("xa", [128, S], F32)
            gg = sb("gg", [128, S], F32)
            xab = sb("xab", [128, S], BF16)
            rr_ = [sb(f"rr{i}", [128, S], F32) for i in range(2)]
            ig_ = [sb(f"ig{i}", [128, S], F32) for i in range(2)]
            aa_ = [sb(f"aa{i}", [128, S], F32) for i in range(2)]
            bx = sb("bx", [128, S], F32)
            hs = [sb(f"hs{i}", [128, S], F32) for i in range(2)]
            mo = sb("mo", [128, S], BF16)
            for d in range(2):
                for dst, srcd, kn in ((lam, lru_lambda, "lam"), (br, lru_b_r, "br"), (bi, lru_b_i, "bi")):
                    P.dma("sp", "c" + kn, lambda e, dst=dst, srcd=srcd, d=d: e.dma_start(
                        out=dst[:, d, :], in_=srcd[d:d + 1, :].rearrange("o (g c) -> c (o g)", c=128), allow_slow_non_contiguous=True), w=[kn])
            P.op("pool", lambda e: e.memset(bd[:], 0.0), w=["bd"])
            nb_ = 0
            for g in range(8):
                for d in range(2):
                    for kind, wsrc in enumerate((lru_w_r, lru_w_i)):
                        for hl in range(2):
                            nb_ += 1
                            P.dma("pool", f"bdl{nb_ % 4}", lambda e, g=g, d=d, kind=kind, wsrc=wsrc, hl=hl: e.dma_start(
                                out=bd[hl * 64:(hl + 1) * 64, g * 4 + d * 2 + kind, hl * 64:(hl + 1) * 64], in_=wsrc[d, 2 * g + hl, :, :]), r=["bd"], w=[f"bdx{nb_}"])
            bdkeys = [f"bdx{i}" for i in range(1, nb_ + 1)]
            P.op("act", lambda e: e.activation(out=lamc[:], in_=lam[:], func=AF.Exp, scale=-1.0), r=["lam"], w=["lamc"])
            P.op("act", lambda e: e.activation(out=lamc[:], in_=lamc[:], func=AF.Ln, bias=1.0, scale=1.0), r=["lamc"], w=["lamc"])
            P.op("dve", lambda e: e.tensor_scalar(out=lamc2[:], in0=lamc[:], scalar1=-16.0, scalar2=None, op0=ALU.mult), r=["lamc"], w=["lamc2"])
            P.op("dve", lambda e: e.tensor_scalar(out=lamc[:], in0=lamc[:], scalar1=-8.0, scalar2=None, op0=ALU.mult), r=["lamc", "lamc2"], w=["lamc"])
            first = True
            for g in range(8):
                P.dma("sp", "xa", lambda e, g=g: e.dma_start(out=xa[:], in_=projT[g * 128:(g + 1) * 128, :]), w=["xa"])
                P.dma("sp", "gg", lambda e, g=g: e.dma_start(out=gg[:], in_=projT[(8 + g) * 128:(9 + g) * 128, :]), w=["gg"])
                P.op("act", lambda e: e.activation(out=xab[:], in_=xa[:], func=AF.Identity), r=["xa"], w=["xab"])
                for d in range(2):
                    rr = rr_[d]
                    ig = ig_[d]
                    aa = aa_[d]
                    krr, kig, kaa = f"rr{d}", f"ig{d}", f"aa{d}"
                    for kind, dst, bias, dk in ((1, ig, bi, kig), (0, rr, br, krr)):
                        for tc_ in range(8):
                            bank = nbank()
                            P.op("pe", lambda e, bank=bank, g=g, d=d, kind=kind, tc_=tc_: e.matmul(
                                ps[bank][:], lhsT=bd[:, g * 4 + d * 2 + kind, :], rhs=xab[:, tc_ * 512:(tc_ + 1) * 512], start=True, stop=True),
                                r=["xab"] + (bdkeys if first else []), w=[f"ps{bank}"])
                            first = False
                            P.op("act", lambda e, bank=bank, dst=dst, bias=bias, d=d, g=g, tc_=tc_: e.activation(
                                out=dst[:, tc_ * 512:(tc_ + 1) * 512], in_=ps[bank][:], func=AF.Sigmoid, bias=bias[:, d, g:g + 1]),
                                r=[f"ps{bank}", "br", "bi"], w=[dk])
                    P.op("dve", lambda e, ig=ig: e.tensor_tensor(out=bx[:], in0=ig[:], in1=xa[:], op=ALU.mult), r=[kig, "xa"], w=["bx"])
                    P.op("act", lambda e, d=d, g=g, rr=rr, aa=aa: e.activation(out=aa[:], in_=rr[:], func=AF.Exp, scale=lamc[:, d, g:g + 1]), r=[krr, "lamc"], w=[kaa])
                    P.op("act", lambda e, d=d, g=g, rr=rr: e.activation(out=rr[:], in_=rr[:], func=AF.Exp, scale=lamc2[:, d, g:g + 1]), r=[krr, "lamc2"], w=[krr])
                    P.op("dve", lambda e, rr=rr: e.tensor_scalar(out=rr[:], in0=rr[:], scalar1=1.0, scalar2=-1.0, op0=ALU.min, op1=ALU.mult), r=[krr], w=[krr])
                    P.op("act", lambda e, rr=rr: e.activation(out=rr[:], in_=rr[:], func=AF.Sqrt, bias=1.0, scale=1.0), r=[krr], w=[krr])
                    st_ = 0 if d == 0 else S - 1
                    P.op("dve", lambda e, st_=st_, rr=rr: e.memset(rr[:, st_:st_ + 1], 1.0), r=[krr], w=[krr])
                    P.op("dve", lambda e, rr=rr: e.tensor_tensor(out=bx[:], in0=bx[:], in1=rr[:], op=ALU.mult), r=["bx", krr], w=["bx"])
                    if d == 0:
                        P.op("dve", lambda e, aa=aa: e.tensor_tensor_scan(out=hs[0][:], data0=aa[:], data1=bx[:], initial=0.0, op0=ALU.mult, op1=ALU.add),
                             r=[kaa, "bx"], w=["hs0"])
                    else:
                        P.op("dve", lambda e, aa=aa: e.tensor_tensor_scan(out=revap(hs[1][:], S), data0=revap(aa[:], S), data1=revap(bx[:], S), initial=0.0,
                                                                   op0=ALU.mult, op1=ALU.add), r=[kaa, "bx"], w=["hs1"])
                P.op("dve", lambda e: e.tensor_tensor(out=hs[0][:], in0=hs[0][:], in1=hs[1][:], op=ALU.add), r=["hs0", "hs1"], w=["hs0"])
                P.op("dve", lambda e: e.tensor_tensor(out=mo[:], in0=hs[0][:], in1=gg[:], op=ALU.mult), r=["hs0", "gg"], w=["mo"])
                P.dma("sp", "mo", lambda e, g=g: e.dma_start(out=maT[g * 128:(g + 1) * 128, :], in_=mo[:]), r=["mo"], w=[f"maT{g}"])
          P.barrier()

        if upto >= 2:
            phase2()

        def fft_fwd(srcfn, nhi, W0, W1, ft, G, FT1d, ftstream, srckey):
            V = W0[:].rearrange("p (l c) -> p l c", c=128)
            Bv = W1[:].rearrange("p (l c) -> p l c", c=128)
            for j in range(16):
                bank = nbank()
                pst = ps[bank][:].bitcast(BF16)
                for q in range(8):
                    tl = j * 8 + q
                    P.op("pe", lambda e, pst=pst, q=q, tl=tl: e.transpose(pst[0:nhi, q * 128:(q + 1) * 128], srcfn(tl), ident[:]),
                         r=[srckey, "ident"], w=[f"ps{bank}"])
                copy_op(evac_eng(), W0[:].bitcast(F32)[0:nhi, j * 512:(j + 1) * 512], ps[bank][0:nhi, :], [f"ps{bank}"], bk("W0", j * 1024, (j + 1) * 1024))
            for s_ in range(8):
                fb = s_ % 2
                P.dma("sp", f"{ftstream}{fb}", lambda e, fb=fb, s_=s_: e.dma_start(out=ft[fb][0:64, :, :], in_=FT1d[:, s_ * 16:(s_ + 1) * 16, :]), w=[f"ft{fb}"])
                for jj in range(4):
                    j = s_ * 4 + jj
                    bank = nbank()
                    for q in range(4):
                        tl = j * 4 + q
                        P.op("pe", lambda e, bank=bank, q=q, tl=tl, fb=fb: e.matmul(
                            ps[bank][:, q * 128:(q + 1) * 128], lhsT=ft[fb][0:nhi, tl % 16, :], rhs=V[0:nhi, tl, :], start=True, stop=True),
                            r=[f"ft{fb}"] + bk("W0", tl * 128, tl * 128 + 128), w=[f"ps{bank}"])
                    copy_op(evac_eng(), Bv[:, j * 4:(j + 1) * 4, :], ps[bank][:].rearrange("p (a c) -> p a c", a=4), [f"ps{bank}"], bk("W1", j * 512, (j + 1) * 512))
            Cv = W0[:].rearrange("p (c f) -> p c f", f=128)
            for j in range(16):
                bank = nbank()
                pst = ps[bank][:].bitcast(BF16)
                for q in range(8):
                    c_ = j * 8 + q
                    P.op("pe", lambda e, pst=pst, q=q, c_=c_: e.transpose(pst[:, q * 128:(q + 1) * 128], Bv[:, :, c_], ident[:]),
                         r=allk("W1") + ["ident"], w=[f"ps{bank}"])
                copy_op(evac_eng(), W0[:].bitcast(F32)[:, j * 512:(j + 1) * 512], ps[bank][:], [f"ps{bank}"], bk("W0", j * 1024, (j + 1) * 1024))
            Xre = W1[:, 0:8192].rearrange("p (c f) -> p c f", f=64)
            Xim = W1[:, 8192:16384].rearrange("p (c f) -> p c f", f=64)
            for j in range(16):
                rre = Cv[:, j * 8:(j + 1) * 8, 0:64]
                rim = Cv[:, j * 8:(j + 1) * 8, 64:128]
                rk = bk("W0", j * 1024, (j + 1) * 1024) + ["G"]
                b1 = nbank()
                o1 = ps[b1][:].rearrange("p (a f) -> p a f", a=8)
                P.op("pe", lambda e, o1=o1, rre=rre: e.matmul(o1, lhsT=G[:, 0, :], rhs=rre, start=True, stop=False), r=rk, w=[f"ps{b1}"])
                P.op("pe", lambda e, o1=o1, rim=rim: e.matmul(o1, lhsT=G[:, 2, :], rhs=rim, start=False, stop=True), r=rk, w=[f"ps{b1}"])
                copy_op(evac_eng(), Xre[:, j * 8:(j + 1) * 8, :], o1, [f"ps{b1}"], bk("W1", j * 512, (j + 1) * 512))
                b2 = nbank()
                o2 = ps[b2][:].rearrange("p (a f) -> p a f", a=8)
                P.op("pe", lambda e, o2=o2, rre=rre: e.matmul(o2, lhsT=G[:, 1, :], rhs=rre, start=True, stop=False), r=rk, w=[f"ps{b2}"])
                P.op("pe", lambda e, o2=o2, rim=rim: e.matmul(o2, lhsT=G[:, 0, :], rhs=rim, start=False, stop=True), r=rk, w=[f"ps{b2}"])
                copy_op(evac_eng(), Xim[:, j * 8:(j + 1) * 8, :], o2, [f"ps{b2}"], bk("W1", 8192 + j * 512, 8192 + (j + 1) * 512))

        MAGIC = 12582912.0
        TWO_PI = float(2 * np.pi)
        def phase3a():
          with ExitStack() as p3:
            def sb(name, shape, dt):
                return p3.enter_context(nc.sbuf_tensor(name, shape, dt))
            zT = sb("zT", [33, S], F32)
            w1s = sb("w1s", [33, 64], F32)
            w2s = sb("w2s", [64, 64], F32)
            b1s = sb("b1s", [64, 1], F32)
            b2s = sb("b2s", [64, 1], F32)
            fq = sb("fq", [64, 2], F32)
            H1T = sb("H1T", [64, S], F32)
            H2T = sb("H2T", [64, S], F32)
            w3s = sb("w3s", [64, 4096], BF16)
            H2Tb = sb("H2Tb", [64, S], BF16)
            G = sb("G", [128, 3, 128], BF16)
            ndel = sb("ndel", [128, 8], F32)
            nbias = sb("nbias", [128, 8, 8], F32)
            tn = sb("tn", [128, 512], F32)
            fbias = sb("fbias", [128, 2, 8], F32)
            ft = [sb(f"ft{i}", [64, 16, 128], BF16) for i in range(2)]
            W0 = sb("W0", [128, 16384], BF16)
            W1 = sb("W1", [128, 16384], BF16)
            kc = sb("kc", [128, 8192], BF16)
            kc1 = sb("kc1", [128, 8192], BF16)
            dec = [sb(f"dec{i}", [128, 512], F32) for i in range(2)]
            tA = sb("tA", [64, 512], F32)
            tB = sb("tB", [64, 512], F32)
            for dst, srcd, kn in ((zT, c_zT, "zT"), (w1s, filt_w1, "w1s"), (w2s, filt_w2, "w2s"), (b1s, filt_b1, "b1s"), (b2s, filt_b2, "b2s"),
                                  (fq, filt_freq, "fq"), (G, c_G, "G"), (ndel, c_ndel, "ndel"), (tn, c_tn, "tn")):
                P.dma("sp", "k" + kn, lambda e, dst=dst, srcd=srcd: e.dma_start(out=dst[:], in_=srcd), w=[kn])
            P.dma("pool", "kw3s", lambda e: e.dma_start(out=w3s[:], in_=filt_w3), w=["w3s"])
            for n in range(2):
                P.dma("sp", "kfb", lambda e, n=n: e.dma_start(out=fbias[:, n, :], in_=filt_bias[n:n + 1, :].rearrange("o (g c) -> c (o g)", c=128),
                                                            allow_slow_non_contiguous=True), w=["fbias"])
            for j in range(8):
                P.op("dve", lambda e, j=j: e.tensor_scalar(out=nbias[:, :, j], in0=ndel[:], scalar1=float(512 * j / (S - 1)), scalar2=None, op0=ALU.mult),
                     r=["ndel"], w=["nbias"])
            for layer, (wl_, bl_, kdim, srcT, dstT, fcol) in enumerate(((w1s, b1s, 33, zT, H1T, 0), (w2s, b2s, 64, H1T, H2T, 1))):
                for j in range(8):
                    bank = nbank()
                    P.op("pe", lambda e, bank=bank, wl_=wl_, kdim=kdim, srcT=srcT, j=j: e.matmul(
                        ps[bank][0:64, :], lhsT=wl_[0:kdim, :], rhs=srcT[0:kdim, j * 512:(j + 1) * 512], start=True, stop=True),
                        r=["w1s", "w2s", "zT", f"H{layer}"], w=[f"ps{bank}"])
                    P.op("dve", lambda e, bank=bank, bl_=bl_, fcol=fcol: e.tensor_scalar(out=tA[:], in0=ps[bank][0:64, :], scalar1=bl_[:, 0:1], scalar2=fq[:, fcol:fcol + 1],
                                                                                      op0=ALU.add, op1=ALU.mult), r=[f"ps{bank}", "b1s", "b2s", "fq"], w=["tA"])
                    P.op("dve", lambda e: e.tensor_scalar(out=tB[:], in0=tA[:], scalar1=1.0 / TWO_PI, scalar2=MAGIC, op0=ALU.mult, op1=ALU.add), r=["tA"], w=["tB"])
                    P.op("dve", lambda e: e.tensor_scalar(out=tB[:], in0=tB[:], scalar1=-MAGIC, scalar2=None, op0=ALU.add), r=["tB"], w=["tB"])
                    P.op("dve", lambda e: e.scalar_tensor_tensor(out=tA[:], in0=tB[:], scalar=-TWO_PI, in1=tA[:], op0=ALU.mult, op1=ALU.add), r=["tA", "tB"], w=["tA"])
                    P.op("act", lambda e, dstT=dstT, j=j: e.activation(out=dstT[:, j * 512:(j + 1) * 512], in_=tA[:], func=AF.Sin, scale=1.0 - 2e-6),
                         r=["tA"], w=[f"H{layer + 1}"])
            if debug:
                P.dma("sp", "h2d", lambda e: e.dma_start(out=H2dbg, in_=H2T[:]), r=["H2"], w=["H2dbg"])
            P.op("dve", lambda e: e.tensor_copy(out=H2Tb[:], in_=H2T[:]), r=["H2"], w=["H2b"])
            kcs = [kc, kc1]

            def gen_filter(idx):
                g, n = idx // 2, idx % 2
                kcb = kcs[idx % 2]
                fk = f"fsrc{idx % 2}"
                P.op("pool", lambda e: e.memset(kcb[:, 4096:4097], 0.0), r=[fk], w=[fk])
                for j in range(8):
                    db = j % 2
                    P.op("act", lambda e, db=db, j=j: e.activation(out=dec[db][:], in_=tn[:], func=AF.Exp, scale=ndel[:, g:g + 1], bias=nbias[:, g, j:j + 1]),
                         r=["tn", "ndel", "nbias"], w=[f"dec{db}"])
                    for dirn in range(2):
                        col0 = n * 2048 + dirn * 1024 + g * 128
                        bank = nbank()
                        P.op("pe", lambda e, bank=bank, col0=col0, j=j: e.matmul(ps[bank][:], lhsT=w3s[0:64, col0:col0 + 128], rhs=H2Tb[0:64, j * 512:(j + 1) * 512],
                                                                                 start=True, stop=True), r=["w3s", "H2b"], w=[f"ps{bank}"])
                        if dirn == 0:
                            P.op("dve", lambda e, bank=bank, db=db, j=j: e.tensor_tensor(out=kcb[:, j * 512:(j + 1) * 512], in0=ps[bank][:], in1=dec[db][:], op=ALU.mult),
                                 r=[f"ps{bank}", f"dec{db}"], w=[fk])
                        else:
                            lo = 1 if j == 0 else 0
                            nel = 512 - lo
                            p0 = 8192 - (j * 512 + 511)
                            P.op("dve", lambda e, bank=bank, db=db, lo=lo, nel=nel, p0=p0: e.tensor_tensor(
                                out=kcb[:, p0:p0 + nel], in0=revap(ps[bank][:, lo:512], nel), in1=revap(dec[db][:, lo:512], nel), op=ALU.mult),
                                r=[f"ps{bank}", f"dec{db}"], w=[fk])
                P.op("dve", lambda e: e.tensor_scalar(out=kcb[:, 0:1], in0=kcb[:, 0:1], scalar1=fbias[:, n, g:g + 1], scalar2=None, op0=ALU.add),
                     r=[fk, "fbias"], w=[fk])

            def fft_filter(idx):
                kcb = kcs[idx % 2]
                kv = kcb[:].rearrange("c (h l) -> c l h", l=128)
                fft_fwd(lambda tl, kv=kv: kv[:, tl, 0:64], 64, W0, W1, ft, G, c_FT1, "ftA", f"fsrc{idx % 2}")
                P.dma("sp", "ksp", lambda e: e.dma_start(out=Kspec[idx], in_=W1[:].rearrange("p (r x) -> p r x", r=2)),
                      r=allk("W1"), w=[f"Kspec{idx}"])
            gen_filter(0)
            for idx in range(16):
                if idx + 1 < 16:
                    gen_filter(idx + 1)
                fft_filter(idx)
          P.barrier()

        if upto >= 3:
            phase3a()

        def phase3b():
          with ExitStack() as p4:
            def sb(name, shape, dt):
                return p4.enter_context(nc.sbuf_tensor("b_" + name, shape, dt))
            G = sb("G", [128, 3, 128], BF16)
            MI = sb("MI", [128, 128, 32], BF16)
            ft = [sb(f"ft{i}", [64, 16, 128], BF16) for i in range(2)]
            W0 = sb("W0", [128, 16384], BF16)
            W1 = sb("W1", [128, 16384], BF16)
            Kc = [sb(f"Kc{i}", [128, 2, 4096], BF16) for i in range(2)]
            tmps = [sb(f"tm{i}", [128, 4096], BF16) for i in range(4)]
            ub = [sb(f"ub{i}", [128, S], BF16) for i in range(2)]
            gate = sb("gate", [128, S], F32)
            mo = sb("mo2", [128, S], BF16)
            P.dma("sp", "kG", lambda e: e.dma_start(out=G[:], in_=c_G), w=["G"])
            P.dma("sp", "kMI", lambda e: e.dma_start(out=MI[:], in_=c_MI), w=["MI"])
            kq = 0
            for g in range(8):
                P.dma("pool", "uld", lambda e, g=g: e.dma_start(out=ub[0][:], in_=projT[(16 + g) * 128:(17 + g) * 128, :]), w=["fsrc0"])
                for n in range(2):
                    usrc = ub[n]
                    udst = ub[1] if n == 0 else mo
                    udk = "fsrc1" if n == 0 else "mo2"
                    grow = (24 + g) if n == 0 else (32 + g)
                    P.dma("sp", "gld", lambda e, grow=grow: e.dma_start(out=gate[:], in_=projT[grow * 128:(grow + 1) * 128, :]), w=["gate"])
                    uv = usrc[:].rearrange("c (h l) -> c l h", l=128)
                    fft_fwd(lambda tl, uv=uv: uv[:, tl, 0:32], 32, W0, W1, ft, G, c_FT1, "ftB", f"fsrc{n}")
                    for q in range(2):
                        kb = kq % 2
                        kq += 1
                        P.dma("sp", f"kc{kb}", lambda e, kb=kb, g=g, n=n, q=q: e.dma_start(
                            out=Kc[kb][:], in_=Kspec[g * 2 + n][:, :, q * 4096:(q + 1) * 4096]), w=[f"Kc{kb}"])
                        xre = W1[:, q * 4096:(q + 1) * 4096]
                        xim = W1[:, 8192 + q * 4096:8192 + (q + 1) * 4096]
                        yre = W0[:, q * 4096:(q + 1) * 4096]
                        yim = W0[:, 8192 + q * 4096:8192 + (q + 1) * 4096]
                        kre = Kc[kb][:, 0, :]
                        kim = Kc[kb][:, 1, :]
                        xrk = bk("W1", q * 4096, (q + 1) * 4096)
                        xik = bk("W1", 8192 + q * 4096, 8192 + (q + 1) * 4096)
                        yrk = bk("W0", q * 4096, (q + 1) * 4096)
                        yik = bk("W0", 8192 + q * 4096, 8192 + (q + 1) * 4096)
                        P.op("dve", lambda e, xre=xre, kre=kre: e.tensor_tensor(out=tmps[0][:], in0=xre, in1=kre, op=ALU.mult), r=xrk + [f"Kc{kb}"], w=["tm0"])
                        P.op("dve", lambda e, xim=xim, kim=kim: e.tensor_tensor(out=tmps[1][:], in0=xim, in1=kim, op=ALU.mult), r=xik + [f"Kc{kb}"], w=["tm1"])
                        P.op("dve", lambda e, yre=yre: e.tensor_tensor(out=yre, in0=tmps[0][:], in1=tmps[1][:], op=ALU.subtract), r=["tm0", "tm1"], w=yrk)
                        P.op("dve", lambda e, xre=xre, kim=kim: e.tensor_tensor(out=tmps[2][:], in0=xre, in1=kim, op=ALU.mult), r=xrk + [f"Kc{kb}"], w=["tm2"])
                        P.op("dve", lambda e, xim=xim, kre=kre: e.tensor_tensor(out=tmps[3][:], in0=xim, in1=kre, op=ALU.mult), r=xik + [f"Kc{kb}"], w=["tm3"])
                        P.op("dve", lambda e, yim=yim: e.tensor_tensor(out=yim, in0=tmps[2][:], in1=tmps[3][:], op=ALU.add), r=["tm2", "tm3"], w=yik)
                    Yre = W0[:, 0:8192].rearrange("p (c f) -> p c f", f=64)
                    Yim = W0[:, 8192:16384].rearrange("p (c f) -> p c f", f=64)
                    Dv = W1[:].rearrange("p (c f) -> p c f", f=128)
                    for j in range(16):
                        rre = Yre[:, j * 8:(j + 1) * 8, :]
                        rim = Yim[:, j * 8:(j + 1) * 8, :]
                        rk = bk("W0", j * 512, (j + 1) * 512) + bk("W0", 8192 + j * 512, 8192 + (j + 1) * 512) + ["G"]
                        wk = bk("W1", j * 1024, (j + 1) * 1024)
                        b1 = nbank()
                        o1 = ps[b1][:].rearrange("p (a f) -> p a f", a=8)
                        P.op("pe", lambda e, o1=o1, rre=rre: e.matmul(o1, lhsT=G[:, 0, :], rhs=rre, start=True, stop=False), r=rk, w=[f"ps{b1}"])
                        P.op("pe", lambda e, o1=o1, rim=rim: e.matmul(o1, lhsT=G[:, 1, :], rhs=rim, start=False, stop=True), r=rk, w=[f"ps{b1}"])
                        copy_op(evac_eng(), Dv[:, j * 8:(j + 1) * 8, 0:64], o1, [f"ps{b1}"], wk)
                        b2 = nbank()
                        o2 = ps[b2][:].rearrange("p (a f) -> p a f", a=8)
                        P.op("pe", lambda e, o2=o2, rre=rre: e.matmul(o2, lhsT=G[:, 2, :], rhs=rre, start=True, stop=False), r=rk, w=[f"ps{b2}"])
                        P.op("pe", lambda e, o2=o2, rim=rim: e.matmul(o2, lhsT=G[:, 0, :], rhs=rim, start=False, stop=True), r=rk, w=[f"ps{b2}"])
                        copy_op(evac_eng(), Dv[:, j * 8:(j + 1) * 8, 64:128], o2, [f"ps{b2}"], wk)
                    Ev = W0[:].rearrange("p (c l) -> p l c", l=128)
                    EvT = W0[:].rearrange("p (l c) -> p c l", c=128)
                    for j in range(16):
                        bank = nbank()
                        pst = ps[bank][:].bitcast(BF16)
                        for q in range(8):
                            c_ = j * 8 + q
                            P.op("pe", lambda e, pst=pst, q=q, c_=c_: e.transpose(pst[:, q * 128:(q + 1) * 128], Dv[:, c_, :], ident[:]),
                                 r=bk("W1", c_ * 128, c_ * 128 + 128) + ["ident"], w=[f"ps{bank}"])
                        copy_op(evac_eng(), W0[:].bitcast(F32)[:, j * 512:(j + 1) * 512], ps[bank][:], [f"ps{bank}"], bk("W0", j * 1024, (j + 1) * 1024))
                    uo = udst[:].rearrange("c (h l) -> c h l", l=128)
                    gv = gate[:].rearrange("c (h l) -> c h l", l=128)
                    for j in range(8):
                        bank = nbank()
                        psv = ps[bank][:].rearrange("p (h q) -> p h q", q=16)
                        for q in range(16):
                            tl = j * 16 + q
                            P.op("pe", lambda e, psv=psv, q=q, tl=tl: e.matmul(psv[:, :, q], lhsT=Ev[:, tl, :], rhs=MI[:, tl, :], start=True, stop=True),
                                 r=allk("W0") + ["MI"], w=[f"ps{bank}"])
                        P.op("dve", lambda e, psv=psv, j=j, uo=uo, gv=gv: e.tensor_tensor(
                            out=uo[:, :, j * 16:(j + 1) * 16], in0=psv, in1=gv[:, :, j * 16:(j + 1) * 16], op=ALU.mult),
                            r=[f"ps{bank}", "gate"], w=[udk])
                P.dma("sp", "mo2", lambda e, g=g: e.dma_start(out=mbT[g * 128:(g + 1) * 128, :], in_=mo[:]), r=["mo2"], w=[f"mbT{g}"])
          P.barrier()
        if upto >= 4:
            phase3b()

        def bcast_last(t_ap, n):
            a = t_ap.ap
            return bass.AP(t_ap.tensor, t_ap.offset, [list(x) for x in a] + [[0, n]])

        def phase4():
          with ExitStack() as p5:
            def sb(name, shape, dt):
                return p5.enter_context(nc.sbuf_tensor("d_" + name, shape, dt))
            waT = sb("waT", [128, 8, 1024], BF16)
            wbT = sb("wbT", [128, 8, 1024], BF16)
            woT = sb("woT", [128, 8, 1024], BF16)
            mA = sb("mA", [128, 8, 512], BF16)
            mB = sb("mB", [128, 8, 512], BF16)
            gA = sb("gA", [128, 8, 512], F32)
            gBt = sb("gBt", [128, 8, 512], F32)
            t1 = sb("t1", [128, 512], F32)
            t2 = sb("t2", [128, 512], F32)
            mg = sb("mg", [128, 8, 512], BF16)
            xt = sb("xt", [128, 1024], F32)
            x1t = sb("x1t", [128, 1024], F32)
            sq = sb("sq", [128, 1024], F32)
            h2f = sb("h2f", [128, 1024], F32)
            h2b = sb("h2b", [128, 1024], BF16)
            h2T = sb("h2T", [128, 8, 128], F32)
            gF = sb("gF", [128, 1024], F32)
            wr = sb("wr", [128, 8, 16], F32)
            ss = sb("ss", [128, 32], F32)
            rstd = sb("rstd", [128, 32], F32)
            for dst, srcw, kn in ((waT, w_a_out, "waT"), (wbT, w_b_out, "wbT"), (woT, w_o, "woT")):
                P.dma("pool", "L" + kn, lambda e, dst=dst, srcw=srcw: e.dma_start(out=dst[:], in_=srcw.rearrange("(a p) d -> p a d", p=128)), w=[kn])
            P.dma("sp", "LgF", lambda e: e.dma_start(out=gF[:], in_=g_ffn.partition_broadcast(128)), w=["gF"])
            P.dma("sp", "Lwr", lambda e: e.dma_start(out=wr[:], in_=w_router.rearrange("(a p) e -> p a e", p=128)), w=["wr"])
            P.op("dve", lambda e: e.memset(ss[:], 0.0), w=["ss"])
            for tc_ in range(8):
                sl = slice(tc_ * 512, (tc_ + 1) * 512)
                P.dma("sp", "LmA", lambda e, sl=sl: e.dma_start(out=mA[:], in_=maT.rearrange("(a p) t -> p a t", p=128)[:, :, sl]), w=["mA"])
                P.dma("sp", "LmB", lambda e, sl=sl: e.dma_start(out=mB[:], in_=mbT.rearrange("(a p) t -> p a t", p=128)[:, :, sl]), w=["mB"])
                P.dma("sp", "LgA", lambda e, sl=sl: e.dma_start(out=gA[:], in_=projT[5120:6144, :].rearrange("(a p) t -> p a t", p=128)[:, :, sl]), w=["gA"])
                P.dma("sp", "LgB", lambda e, sl=sl: e.dma_start(out=gBt[:], in_=projT[6144:7168, :].rearrange("(a p) t -> p a t", p=128)[:, :, sl]), w=["gBt"])
                for dc in range(8):
                    ba = nbank()
                    for kk in range(8):
                        P.op("pe", lambda e, ba=ba, kk=kk, dc=dc: e.matmul(ps[ba][:], lhsT=waT[:, kk, dc * 128:(dc + 1) * 128], rhs=mA[:, kk, :], start=(kk == 0), stop=(kk == 7)),
                             r=["waT", "mA"], w=[f"ps{ba}"])
                    bb = nbank()
                    for kk in range(8):
                        P.op("pe", lambda e, bb=bb, kk=kk, dc=dc: e.matmul(ps[bb][:], lhsT=wbT[:, kk, dc * 128:(dc + 1) * 128], rhs=mB[:, kk, :], start=(kk == 0), stop=(kk == 7)),
                             r=["wbT", "mB"], w=[f"ps{bb}"])
                    P.op("dve", lambda e, ba=ba, dc=dc: e.tensor_tensor(out=t1[:], in0=ps[ba][:], in1=gA[:, dc, :], op=ALU.mult), r=[f"ps{ba}", "gA"], w=["t1"])
                    P.op("dve", lambda e, bb=bb, dc=dc: e.tensor_tensor(out=t2[:], in0=ps[bb][:], in1=gBt[:, dc, :], op=ALU.mult), r=[f"ps{bb}", "gBt"], w=["t2"])
                    P.op("dve", lambda e, dc=dc: e.tensor_tensor(out=mg[:, dc, :], in0=t1[:], in1=t2[:], op=ALU.add), r=["t1", "t2"], w=["mg"])
                for tq in range(4):
                    tt = tc_ * 4 + tq
                    P.dma("sp", "Lxt", lambda e, tt=tt: e.dma_start(out=xt[:], in_=x[tt * 128:(tt + 1) * 128, :]), w=["xt"])
                    for half in range(2):
                        bo = nbank()
                        for kk in range(8):
                            P.op("pe", lambda e, bo=bo, kk=kk, tq=tq, half=half: e.matmul(ps[bo][:], lhsT=mg[:, kk, tq * 128:(tq + 1) * 128], rhs=woT[:, kk, half * 512:(half + 1) * 512],
                                                                                         start=(kk == 0), stop=(kk == 7)), r=["mg", "woT"], w=[f"ps{bo}"])
                        P.op("dve", lambda e, bo=bo, half=half: e.tensor_tensor(out=x1t[:, half * 512:(half + 1) * 512], in0=ps[bo][:], in1=xt[:, half * 512:(half + 1) * 512], op=ALU.add),
                             r=[f"ps{bo}", "xt"], w=["x1t"])
                    P.dma("sp", "Sx1", lambda e, tt=tt: e.dma_start(out=x1d[tt * 128:(tt + 1) * 128, :], in_=x1t[:]), r=["x1t"], w=[f"x1d{tt}"])
                    P.op("act", lambda e, tt=tt: e.activation(out=sq[:], in_=x1t[:], func=AF.Square, accum_out=ss[:, tt:tt + 1]), r=["x1t", "ss"], w=["sq", f"ss{tt}"])
                    P.op("act", lambda e, tt=tt: e.activation(out=rstd[:, tt:tt + 1], in_=ss[:, tt:tt + 1], func=AF.Sqrt, bias=EPS, scale=1.0 / D), r=[f"ss{tt}"], w=[f"rs{tt}"])
                    P.op("dve", lambda e, tt=tt: e.reciprocal(out=rstd[:, tt:tt + 1], in_=rstd[:, tt:tt + 1]), r=[f"rs{tt}"], w=[f"rs{tt}"])
                    P.op("dve", lambda e, tt=tt: e.scalar_tensor_tensor(out=h2f[:], in0=x1t[:], scalar=rstd[:, tt:tt + 1], in1=gF[:], op0=ALU.mult, op1=ALU.mult),
                         r=["x1t", f"rs{tt}", "gF"], w=["h2f"])
                    P.op("act", lambda e: e.activation(out=h2b[:], in_=h2f[:], func=AF.Identity), r=["h2f"], w=["h2b"])
                    P.dma("sp", "Sh2", lambda e, tt=tt: e.dma_start(out=h2d[tt * 128:(tt + 1) * 128, :], in_=h2b[:]), r=["h2b"], w=[f"h2d{tt}"])
                    b0 = nbank()
                    b1_ = nbank()
                    for dh in range(8):
                        bx_ = b0 if dh < 4 else b1_
                        P.op("pe", lambda e, bx_=bx_, dh=dh: e.transpose(ps[bx_][:, (dh % 4) * 128:(dh % 4 + 1) * 128], h2f[:, dh * 128:(dh + 1) * 128], identf[:]),
                             r=["h2f", "identf"], w=[f"ps{bx_}"])
                    P.op("act", lambda e, b0=b0: e.activation(out=h2T[:, 0:4, :], in_=ps[b0][:].rearrange("p (a t) -> p a t", a=4), func=AF.Identity), r=[f"ps{b0}"], w=["h2Ta"])
                    P.op("act", lambda e, b1_=b1_: e.activation(out=h2T[:, 4:8, :], in_=ps[b1_][:].rearrange("p (a t) -> p a t", a=4), func=AF.Identity), r=[f"ps{b1_}"], w=["h2Tb"])
                    bl = nbank()
                    for kk in range(8):
                        P.op("pe", lambda e, bl=bl, kk=kk: e.matmul(ps[bl][:, 0:16], lhsT=h2T[:, kk, :], rhs=wr[:, kk, :], start=(kk == 0), stop=(kk == 7)),
                             r=["h2Ta", "h2Tb", "wr"], w=[f"ps{bl}"])
                    P.op("dve", lambda e, bl=bl, tt=tt: e.tensor_copy(out=lg[:, tt, :], in_=ps[bl][:, 0:16]), r=[f"ps{bl}"], w=["lg"])
          P.barrier()

        def phase5():
          with ExitStack() as p6:
            def sb(name, shape, dt):
                return p6.enter_context(nc.sbuf_tensor("e_" + name, shape, dt))
            mx = sb("mx", [128, 32], F32)
            sm = sb("sm", [128, 32], F32)
            aff = sb("aff", [128, 32, 16], F32)
            affE = sb("affE", [16, S], F32)
            junk = sb("junk", [16, S], F32)
            ones = sb("ones", [16, S], F32)
            csum = sb("csum", [16, S], F32)
            lo = sb("lo", [16, 1], F32)
            hi = sb("hi", [16, 1], F32)
            mid = sb("mid", [16, 1], F32)
            cnt = sb("cnt", [16, 1], F32)
            flag = sb("flag", [16, 1], F32)
            ta = sb("ta", [16, 1], F32)
            tb = sb("tb", [16, 1], F32)
            cT = sb("cT", [128, 32, 16], F32)
            Rall = sb("Rall", [128, 32, 16, 5], BF16)
            tokab = sb("tokab", [128, 32, 2], F32)
            rf = sb("rf", [128, 32, 16], F32)
            r1 = sb("r1", [128, 32, 16], F32)
            iota1 = sb("iota1", [128, 512], F32)
            Pt = [sb(f"Pt{i}", [128, 512], BF16) for i in range(4)]
            P.dma("sp", "Lio", lambda e: e.dma_start(out=iota1[:], in_=c_iota1), w=["iota1"])
            P.dma("sp", "Ltk", lambda e: e.dma_start(out=tokab[:], in_=c_tokab), w=["tokab"])
            P.op("dve", lambda e: e.tensor_reduce(out=mx[:], in_=lg[:], axis=mybir.AxisListType.X, op=ALU.max), r=["lg"], w=["mx"])
            P.op("dve", lambda e: e.tensor_tensor(out=aff[:], in0=lg[:], in1=bcast_last(mx[:], 16), op=ALU.subtract), r=["lg", "mx"], w=["aff"])
            P.op("act", lambda e: e.activation(out=aff[:], in_=aff[:], func=AF.Exp), r=["aff"], w=["aff"])
            P.op("dve", lambda e: e.tensor_reduce(out=sm[:], in_=aff[:], axis=mybir.AxisListType.X, op=ALU.add), r=["aff"], w=["sm"])
            P.op("dve", lambda e: e.reciprocal(out=sm[:], in_=sm[:]), r=["sm"], w=["sm"])
            P.op("dve", lambda e: e.tensor_tensor(out=aff[:], in0=aff[:], in1=bcast_last(sm[:], 16), op=ALU.mult), r=["aff", "sm"], w=["aff"])
            for j in range(8):
                bank = nbank()
                for q in range(4):
                    tt = j * 4 + q
                    P.op("pe", lambda e, bank=bank, q=q, tt=tt: e.transpose(ps[bank][0:16, q * 128:(q + 1) * 128], aff[:, tt, :], identf[:]), r=["aff", "identf"], w=[f"ps{bank}"])
                P.op("act", lambda e, bank=bank, j=j: e.activation(out=affE[:, j * 512:(j + 1) * 512], in_=ps[bank][0:16, :], func=AF.Identity), r=[f"ps{bank}"], w=["affE"])
            P.op("dve", lambda e: e.memset(lo[:], 0.0), w=["lo"])
            P.op("dve", lambda e: e.memset(hi[:], 1.0), w=["hi"])
            P.op("pool", lambda e: e.memset(ones[:], 1.0), w=["ones"])
            D_ = "dve"
            for it in range(32):
                P.op(D_, lambda e: e.tensor_tensor(out=mid[:], in0=lo[:], in1=hi[:], op=ALU.add), r=["lo", "hi"], w=["mid"])
                P.op(D_, lambda e: e.tensor_scalar(out=mid[:], in0=mid[:], scalar1=0.5, scalar2=None, op0=ALU.mult), r=["mid"], w=["mid"])
                P.op(D_, lambda e: e.memset(cnt[:], 0.0), r=["cnt"], w=["cnt"])
                P.op("act", lambda e: e.activation(out=junk[:], in_=affE[:], func=AF.Sign, bias=mid[:, 0:1], scale=-1.0, accum_out=cnt[:, 0:1]), r=["affE", "mid", "cnt"], w=["junk", "cnt"])
                P.op(D_, lambda e: e.tensor_scalar(out=flag[:], in0=cnt[:], scalar1=3072.5, scalar2=None, op0=ALU.is_lt), r=["cnt"], w=["flag"])
                P.op(D_, lambda e: e.tensor_tensor(out=ta[:], in0=mid[:], in1=flag[:], op=ALU.mult), r=["mid", "flag"], w=["ta"])
                P.op(D_, lambda e: e.tensor_tensor(out=lo[:], in0=lo[:], in1=ta[:], op=ALU.max), r=["lo", "ta"], w=["lo"])
                P.op(D_, lambda e: e.tensor_scalar(out=tb[:], in0=flag[:], scalar1=-1.0, scalar2=1.0, op0=ALU.mult, op1=ALU.add), r=["flag"], w=["tb"])
                P.op(D_, lambda e: e.tensor_tensor(out=tb[:], in0=tb[:], in1=mid[:], op=ALU.mult), r=["tb", "mid"], w=["tb"])
                P.op(D_, lambda e: e.scalar_tensor_tensor(out=tb[:], in0=flag[:], scalar=2.0, in1=tb[:], op0=ALU.mult, op1=ALU.add), r=["flag", "tb"], w=["tb"])
                P.op(D_, lambda e: e.tensor_tensor(out=hi[:], in0=hi[:], in1=tb[:], op=ALU.min), r=["hi", "tb"], w=["hi"])
            P.op(D_, lambda e: e.tensor_scalar(out=junk[:], in0=affE[:], scalar1=lo[:, 0:1], scalar2=None, op0=ALU.is_gt), r=["affE", "lo"], w=["junk"])
            P.op(D_, lambda e: e.tensor_tensor_scan(out=csum[:], data0=ones[:], data1=junk[:], initial=0.0, op0=ALU.mult, op1=ALU.add), r=["ones", "junk"], w=["csum"])
            P.op(D_, lambda e: e.tensor_tensor(out=csum[:], in0=csum[:], in1=junk[:], op=ALU.mult), r=["csum", "junk"], w=["csum"])
            bank = nbank()
            for tt in range(32):
                P.op("pe", lambda e, bank=bank, tt=tt: e.transpose(ps[bank][:, tt * 16:(tt + 1) * 16], csum[0:16, tt * 128:(tt + 1) * 128], identf[0:16, 0:16]),
                     r=["csum", "identf"], w=[f"ps{bank}"])
            cTi = sb("cTi", [128, 32, 16], I32)
            P.op("act", lambda e, bank=bank: e.activation(out=cT[:], in_=ps[bank][:].rearrange("p (t x) -> p t x", x=16), func=AF.Identity, bias=0.25), r=[f"ps{bank}"], w=["cT"])
            P.op("dve", lambda e: e.tensor_copy(out=cTi[:], in_=cT[:]), r=["cT"], w=["cTi"])
            P.op("dve", lambda e: e.tensor_copy(out=cT[:], in_=cTi[:]), r=["cTi"], w=["cT"])
            P.op("pool", lambda e: e.tensor_copy(out=Rall[:, :, :, 0], in_=bcast_last(tokab[:, :, 0], 16)), r=["tokab"], w=["Rall0"])
            P.op("pool", lambda e: e.tensor_copy(out=Rall[:, :, :, 1], in_=bcast_last(tokab[:, :, 1], 16)), r=["tokab"], w=["Rall0"])
            P.op("pool", lambda e: e.tensor_copy(out=Rall[:, :, :, 2], in_=aff[:]), r=["aff"], w=["Rall1"])
            P.op("pool", lambda e: e.tensor_copy(out=rf[:], in_=Rall[:, :, :, 2]), r=["Rall1"], w=["rf"])
            P.op("pool", lambda e: e.tensor_tensor(out=r1[:], in0=aff[:], in1=rf[:], op=ALU.subtract), r=["aff", "rf"], w=["r1"])
            P.op("pool", lambda e: e.tensor_copy(out=Rall[:, :, :, 3], in_=r1[:]), r=["r1"], w=["Rall1"])
            P.op("pool", lambda e: e.tensor_copy(out=rf[:], in_=Rall[:, :, :, 3]), r=["Rall1"], w=["rf"])
            P.op("pool", lambda e: e.tensor_tensor(out=r1[:], in0=r1[:], in1=rf[:], op=ALU.subtract), r=["r1", "rf"], w=["r1"])
            P.op("pool", lambda e: e.tensor_copy(out=Rall[:, :, :, 4], in_=r1[:]), r=["r1"], w=["Rall1"])
            bIs = [nbank() for _ in range(4)]
            psIs = [ps[b_][:, 0:80].rearrange("p (x five) -> p x five", x=16) for b_ in bIs]
            pc = 0
            for ex in range(16):
                for tt in range(32):
                    pb_ = pc % 4
                    pc += 1
                    P.op("dve", lambda e, pb_=pb_, tt=tt, ex=ex: e.tensor_scalar(out=Pt[pb_][:], in0=iota1[:], scalar1=cT[:, tt, ex:ex + 1], scalar2=None, op0=ALU.is_equal),
                         r=["iota1", "cT"], w=[f"Pt{pb_}"])
                    for ch in range(4):
                        P.op("pe", lambda e, pb_=pb_, tt=tt, ex=ex, ch=ch: e.matmul(psIs[ch][:, ex, :], lhsT=Pt[pb_][:, ch * 128:(ch + 1) * 128], rhs=Rall[:, tt, ex, :],
                                                                                 start=(tt == 0), stop=(tt == 31)), r=[f"Pt{pb_}", "Rall0", "Rall1"], w=[f"ps{bIs[ch]}"])
            idxF = sb("idxF", [128, 16, 4], F32)
            psS = sb("psS", [128, 4, 16, 5], F32)
            for ch in range(4):
                P.op("dve", lambda e, ch=ch: e.tensor_copy(out=psS[:, ch, :, :], in_=psIs[ch]), r=[f"ps{bIs[ch]}"], w=["psS"])
                P.op("dve", lambda e, ch=ch: e.scalar_tensor_tensor(out=idxF[:, :, ch], in0=psS[:, ch, :, 0], scalar=64.0, in1=psS[:, ch, :, 1], op0=ALU.mult, op1=ALU.add), r=["psS"], w=["idxF"])
                P.op("dve", lambda e, ch=ch: e.tensor_tensor(out=valT[:, :, ch], in0=psS[:, ch, :, 2], in1=psS[:, ch, :, 3], op=ALU.add), r=["psS"], w=["valT"])
                P.op("dve", lambda e, ch=ch: e.tensor_tensor(out=valT[:, :, ch], in0=valT[:, :, ch], in1=psS[:, ch, :, 4], op=ALU.add), r=["psS", "valT"], w=["valT"])
            P.op("dve", lambda e: e.tensor_scalar(out=idxF[:], in0=idxF[:], scalar1=0.25, scalar2=None, op0=ALU.add), r=["idxF"], w=["idxF"])
            P.op("dve", lambda e: e.tensor_copy(out=idxI[:], in_=idxF[:]), r=["idxF"], w=["idxI"])
          P.barrier()

        def phase6():
          with ExitStack() as p7:
            def sb(name, shape, dt):
                return p7.enter_context(nc.sbuf_tensor("f_" + name, shape, dt))
            wgu = [sb(f"wgu{i}", [128, 8, 512], BF16) for i in range(4)]
            wd = [sb(f"wd{i}", [128, 16, 1024], BF16) for i in range(2)]
            xe = sb("xe", [128, 4, 1024], BF16)
            xeT = sb("xeT", [128, 8, 512], BF16)
            actT = sb("actT", [128, 16, 512], BF16)
            sg = sb("sg", [128, 512], F32)
            ye = [sb(f"ye{i}", [128, 1024], F32) for i in range(2)]
            wq = 0
            yq = 0
            for ex in range(16):
                wdb = ex % 2
                for ch in range(4):
                    P.dma("pool", f"ga{ch}", lambda e, ex=ex, ch=ch: e.indirect_dma_start(
                        out=xe[:, ch, :], out_offset=None, in_=h2d[:, :], in_offset=bass.IndirectOffsetOnAxis(ap=idxI[:, ex, ch:ch + 1], axis=0)),
                        r=["idxI"], w=[f"xe{ch}"])
                for ch in range(4):
                    bank = nbank()
                    pst = ps[bank][:].bitcast(BF16)
                    for dh in range(8):
                        P.op("pe", lambda e, pst=pst, dh=dh, ch=ch: e.transpose(pst[:, dh * 128:(dh + 1) * 128], xe[:, ch, dh * 128:(dh + 1) * 128], ident[:]),
                             r=[f"xe{ch}", "ident"], w=[f"ps{bank}"])
                    copy_op(evac_eng(), xeT[:].bitcast(F32)[:, :, ch * 64:(ch + 1) * 64], ps[bank][:].rearrange("p (a s) -> p a s", a=8), [f"ps{bank}"], ["xeT"])
                P.dma("pool", f"Lwd{wdb}", lambda e, ex=ex, wdb=wdb: e.dma_start(out=wd[wdb][:], in_=w_down[ex].rearrange("(a p) d -> p a d", p=128)), w=[f"wd{wdb}"])
                for fc in range(4):
                    gbuf = wq % 4
                    ubuf = (wq + 1) % 4
                    wq += 2
                    P.dma("pool", f"Lw{gbuf}", lambda e, ex=ex, fc=fc, gbuf=gbuf: e.dma_start(
                        out=wgu[gbuf][:], in_=w_gate[ex][:, fc * 512:(fc + 1) * 512].rearrange("(a p) f -> p a f", p=128)), w=[f"wgu{gbuf}"])
                    P.dma("pool", f"Lw{ubuf}", lambda e, ex=ex, fc=fc, ubuf=ubuf: e.dma_start(
                        out=wgu[ubuf][:], in_=w_up[ex][:, fc * 512:(fc + 1) * 512].rearrange("(a p) f -> p a f", p=128)), w=[f"wgu{ubuf}"])
                    for sub in range(4):
                        fi = fc * 4 + sub
                        bg = nbank()
                        for kk in range(8):
                            P.op("pe", lambda e, bg=bg, kk=kk, gbuf=gbuf, sub=sub: e.matmul(ps[bg][:], lhsT=wgu[gbuf][:, kk, sub * 128:(sub + 1) * 128], rhs=xeT[:, kk, :],
                                                                                      start=(kk == 0), stop=(kk == 7)), r=[f"wgu{gbuf}", "xeT"], w=[f"ps{bg}"])
                        bu = nbank()
                        for kk in range(8):
                            P.op("pe", lambda e, bu=bu, kk=kk, ubuf=ubuf, sub=sub: e.matmul(ps[bu][:], lhsT=wgu[ubuf][:, kk, sub * 128:(sub + 1) * 128], rhs=xeT[:, kk, :],
                                                                                      start=(kk == 0), stop=(kk == 7)), r=[f"wgu{ubuf}", "xeT"], w=[f"ps{bu}"])
                        P.op("act", lambda e, bg=bg: e.activation(out=sg[:], in_=ps[bg][:], func=AF.Silu), r=[f"ps{bg}"], w=["sg"])
                        P.op("dve", lambda e, bu=bu, fi=fi: e.tensor_tensor(out=actT[:, fi, :], in0=ps[bu][:], in1=sg[:], op=ALU.mult), r=[f"ps{bu}", "sg"], w=["actT"])
                for ch in range(4):
                    yb_ = yq % 2
                    yq += 1
                    for half in range(2):
                        bo = nbank()
                        for fh in range(16):
                            P.op("pe", lambda e, bo=bo, fh=fh, ch=ch, half=half, wdb=wdb: e.matmul(ps[bo][:], lhsT=actT[:, fh, ch * 128:(ch + 1) * 128], rhs=wd[wdb][:, fh, half * 512:(half + 1) * 512],
                                                                                             start=(fh == 0), stop=(fh == 15)), r=["actT", f"wd{wdb}"], w=[f"ps{bo}"])
                        P.op("dve" if half == 0 else "act",
                             (lambda e, bo=bo, yb_=yb_, half=half, ex=ex, ch=ch: e.tensor_scalar(out=ye[yb_][:, half * 512:(half + 1) * 512], in0=ps[bo][:], scalar1=valT[:, ex, ch:ch + 1], scalar2=None, op0=ALU.mult))
                             if half == 0 else
                             (lambda e, bo=bo, yb_=yb_, half=half, ex=ex, ch=ch: e.activation(out=ye[yb_][:, half * 512:(half + 1) * 512], in_=ps[bo][:], func=AF.Identity, scale=valT[:, ex, ch:ch + 1])),
                             r=[f"ps{bo}", "valT"], w=[f"ye{yb_}"])
                    P.dma("pool", "scat", lambda e, yb_=yb_, ex=ex, ch=ch: e.indirect_dma_start(
                        out=x1d[:, :], out_offset=bass.IndirectOffsetOnAxis(ap=idxI[:, ex, ch:ch + 1], axis=0), in_=ye[yb_][:, :], in_offset=None, compute_op=ALU.add),
                        r=[f"ye{yb_}", "idxI"], w=["x1dall"])
          P.barrier()

        def phase7():
          with ExitStack() as p8:
            def sb(name, shape, dt):
                return p8.enter_context(nc.sbuf_tensor("g_" + name, shape, dt))
            xt = [sb(f"xt{i}", [128, 1024], F32) for i in range(2)]
            ot = [sb(f"ot{i}", [128, 1024], F32) for i in range(2)]
            sq = sb("sq", [128, 1024], F32)
            gF = sb("gF", [128, 1024], F32)
            ss = sb("ss", [128, 32], F32)
            rstd = sb("rstd", [128, 32], F32)
            P.dma("sp", "LgF", lambda e: e.dma_start(out=gF[:], in_=g_final.partition_broadcast(128)), w=["gF"])
            P.op("dve", lambda e: e.memset(ss[:], 0.0), w=["ss"])
            for tt in range(32):
                b = tt % 2
                P.dma("sp", f"Lx{b}", lambda e, b=b, tt=tt: e.dma_start(out=xt[b][:], in_=x1d[tt * 128:(tt + 1) * 128, :]), w=[f"xt{b}"])
                P.op("act", lambda e, b=b, tt=tt: e.activation(out=sq[:], in_=xt[b][:], func=AF.Square, accum_out=ss[:, tt:tt + 1]), r=[f"xt{b}", "ss"], w=["sq", f"ss{tt}"])
                P.op("act", lambda e, tt=tt: e.activation(out=rstd[:, tt:tt + 1], in_=ss[:, tt:tt + 1], func=AF.Sqrt, bias=EPS, scale=1.0 / D), r=[f"ss{tt}"], w=[f"rs{tt}"])
                P.op("dve", lambda e, tt=tt: e.reciprocal(out=rstd[:, tt:tt + 1], in_=rstd[:, tt:tt + 1]), r=[f"rs{tt}"], w=[f"rs{tt}"])
                P.op("dve", lambda e, b=b, tt=tt: e.scalar_tensor_tensor(out=ot[b][:], in0=xt[b][:], scalar=rstd[:, tt:tt + 1], in1=gF[:], op0=ALU.mult, op1=ALU.mult),
                     r=[f"xt{b}", f"rs{tt}", "gF"], w=[f"ot{b}"])
                P.dma("sp", f"So{b}", lambda e, b=b, tt=tt: e.dma_start(out=out[tt * 128:(tt + 1) * 128, :], in_=ot[b][:]), r=[f"ot{b}"], w=[f"out{tt}"])
          P.barrier()

        if upto >= 5:
            phase4()
        if upto >= 6:
            phase5()
        if upto >= 7:
            phase6()
        if upto >= 8:
            phase7()
        k.P = P
        P.emit()
    return nc


_NC_CACHE = {}


def _in_map(inputs, b, consts):
    m = {"x": np.ascontiguousarray(inputs["x"][b])}
    for n in ["g_mix", "w_in", "conv_a_w", "conv_a_b", "lru_w_r", "lru_b_r", "lru_w_i", "lru_b_i", "lru_lambda", "conv_b_w", "conv_b_b",
              "filt_w1", "filt_w2", "filt_w3", "filt_bias", "w_a_out", "w_b_out", "w_o", "g_ffn", "w_router", "w_gate", "w_up", "w_down"]:
        a = np.asarray(inputs[n])[0]
        if a.ndim == 1:
            a = a[None]
        m[n] = np.ascontiguousarray(a.astype(np.float32))
    m["g_final"] = np.ascontiguousarray(np.asarray(inputs["g_final"]).reshape(1, -1).astype(np.float32))
    m["filt_b1"] = np.ascontiguousarray(np.asarray(inputs["filt_b1"])[0].reshape(64, 1))
    m["filt_b2"] = np.ascontiguousarray(np.asarray(inputs["filt_b2"])[0].reshape(64, 1))
    m["filt_freq"] = np.ascontiguousarray(np.asarray(inputs["filt_freq"])[0].T)
    m.update(consts)
    return m


def kernel(**inputs):
    if "nc" not in _NC_CACHE:
        _NC_CACHE["nc"] = build(debug=False)
    nc = _NC_CACHE["nc"]
    consts = make_consts()
    in_maps = [_in_map(inputs, b, consts) for b in range(8)]
    res = run_bass_kernel_spmd(nc, in_maps, core_ids=list(range(8)))
    return np.stack([np.asarray(r["out"]) for r in res.results], axis=0).astype(np.float32)
```
